# Optimizing a Trainium2 kernel written in Bass

```python
import jax, jax.numpy as jnp
from jax import lax
import numpy as np

D_MODEL = 1024
BATCH = 4
SEQ = 8192
DEPTH = 1

D_MIX = D_MODEL
ATTN_HEADS = 4
QK_NOPE_DIM = 128
QK_ROPE_DIM = 64
QK_HEAD_DIM = QK_NOPE_DIM + QK_ROPE_DIM
V_HEAD_DIM = 128
ATTN_WIDTH = ATTN_HEADS * V_HEAD_DIM
Q_LORA_RANK = 256
KV_LORA_RANK = 128
ROPE_THETA = 10000.0
Q_BLOCK = 128
LRU_WIDTH = D_MIX - ATTN_WIDTH
LRU_HEADS = 4
LRU_BLOCK = LRU_WIDTH // LRU_HEADS
CONV_WIDTH = 4
LRU_C = 8.0
IN_SPLITS = [Q_LORA_RANK,
             Q_LORA_RANK + KV_LORA_RANK,
             Q_LORA_RANK + KV_LORA_RANK + QK_ROPE_DIM,
             Q_LORA_RANK + KV_LORA_RANK + QK_ROPE_DIM + LRU_WIDTH]
IN_PROJ_WIDTH = Q_LORA_RANK + KV_LORA_RANK + QK_ROPE_DIM + 2 * LRU_WIDTH
N_GROUPS = 4
EXPERTS_PER_GROUP = 8
N_EXPERTS = N_GROUPS * EXPERTS_PER_GROUP
TOP_K = 2
D_FF_EXPERT = 256
MOE_BLOCK = 128
EPS = 1e-6

kernel_name = 'hymba_mla_rglru_hmoe_adaln_layer'


def rms_norm(x, g):
    xf = x.astype(jnp.float32)
    y = xf * lax.rsqrt(jnp.mean(xf * xf, axis=-1, keepdims=True) + EPS)
    return (y * g.astype(jnp.float32)).astype(x.dtype)


def rope_cos_sin(positions):
    inv = 1.0 / (ROPE_THETA ** (jnp.arange(0, QK_ROPE_DIM, 2, dtype=jnp.float32) / QK_ROPE_DIM))
    ang = positions.astype(jnp.float32)[..., None] * inv
    return jnp.cos(ang), jnp.sin(ang)


def apply_rope(x, cos, sin):
    xf = x.astype(jnp.float32)
    x1, x2 = jnp.split(xf, 2, axis=-1)
    c = cos[:, :, None, :]
    s = sin[:, :, None, :]
    return jnp.concatenate([x1 * c - x2 * s, x2 * c + x1 * s], axis=-1).astype(x.dtype)


def mla_attention(q_lat, kv_lat, k_rope, cos, sin, q_a_norm, w_q_b, kv_a_norm, w_kv_b, q_norm, k_norm):
    B, S, _ = q_lat.shape
    q = (rms_norm(q_lat, q_a_norm) @ w_q_b).reshape(B, S, ATTN_HEADS, QK_HEAD_DIM)
    kv = (rms_norm(kv_lat, kv_a_norm) @ w_kv_b).reshape(B, S, ATTN_HEADS, QK_NOPE_DIM + V_HEAD_DIM)
    k_nope, v = kv[..., :QK_NOPE_DIM], kv[..., QK_NOPE_DIM:]
    k_pe = jnp.broadcast_to(k_rope[:, :, None, :], (B, S, ATTN_HEADS, QK_ROPE_DIM))
    k = jnp.concatenate([k_nope, k_pe], axis=-1)
    q = rms_norm(q, q_norm)
    k = rms_norm(k, k_norm)
    q = jnp.concatenate([q[..., :QK_NOPE_DIM], apply_rope(q[..., QK_NOPE_DIM:], cos, sin)], axis=-1)
    k = jnp.concatenate([k[..., :QK_NOPE_DIM], apply_rope(k[..., QK_NOPE_DIM:], cos, sin)], axis=-1)
    qh = q.transpose(0, 2, 1, 3)
    kh = k.transpose(0, 2, 1, 3)
    vh = v.transpose(0, 2, 1, 3)
    n_blocks = S // Q_BLOCK
    q_blocks = qh.reshape(B, ATTN_HEADS, n_blocks, Q_BLOCK, QK_HEAD_DIM).transpose(2, 0, 1, 3, 4)
    key_idx = jnp.arange(S)
    scale = QK_HEAD_DIM ** -0.5

    def block(args):
        qb, bi = args
        s = jnp.einsum('bhqd,bhkd->bhqk', qb, kh, preferred_element_type=jnp.float32) * scale
        q_idx = bi * Q_BLOCK + jnp.arange(Q_BLOCK)
        s = jnp.where(key_idx[None, :] <= q_idx[:, None], s, -jnp.inf)
        p = jax.nn.softmax(s, axis=-1)
        return jnp.einsum('bhqk,bhkd->bhqd', p.astype(vh.dtype), vh)

    out = lax.map(block, (q_blocks, jnp.arange(n_blocks)))
    return out.transpose(1, 0, 3, 2, 4).reshape(B, S, ATTN_WIDTH)


def rg_lru_branch(x_lru, g_lru, positions, conv_w, conv_b, w_a, b_a, w_x, b_x, lam):
    B, S, C = x_lru.shape
    xc = lax.conv_general_dilated(x_lru, conv_w[:, None, :], window_strides=(1,),
                                  padding=[(CONV_WIDTH - 1, 0)],
                                  dimension_numbers=('NWC', 'WIO', 'NWC'),
                                  feature_group_count=C) + conv_b
    xb = xc.reshape(B, S, LRU_HEADS, LRU_BLOCK)
    r = jax.nn.sigmoid(jnp.einsum('bshi,hij->bshj', xb, w_a).reshape(B, S, C) + b_a)
    i = jax.nn.sigmoid(jnp.einsum('bshi,hij->bshj', xb, w_x).reshape(B, S, C) + b_x)
    log_a = -LRU_C * r.astype(jnp.float32) * jax.nn.softplus(-lam.astype(jnp.float32))
    a = jnp.exp(log_a)
    mult = jnp.sqrt(jnp.maximum(1.0 - jnp.exp(2.0 * log_a), 0.0))
    reset = (positions == 0)[..., None]
    a = jnp.where(reset, 0.0, a)
    mult = jnp.where(reset, 1.0, mult)
    b = mult * (i.astype(jnp.float32) * xc.astype(jnp.float32))

    def combine(lhs, rhs):
        a1, b1 = lhs
        a2, b2 = rhs
        return a1 * a2, a2 * b1 + b2

    _, h = lax.associative_scan(combine, (a, b), axis=1)
    return h.astype(x_lru.dtype) * jax.nn.gelu(g_lru)


def hierarchical_moe(h, w_rg, b_rg, w_re, b_re, w_gate, w_up, w_down):
    B, S, D = h.shape
    N = B * S
    t = h.reshape(N, D)
    g_prob = jax.nn.softmax((t @ w_rg).astype(jnp.float32) + b_rg, axis=-1)
    g_p, g_idx = lax.top_k(g_prob, 1)
    e_logits = ((t @ w_re).astype(jnp.float32) + b_re).reshape(N, N_GROUPS, EXPERTS_PER_GROUP)
    e_logits = jnp.take_along_axis(e_logits, g_idx[:, :, None], axis=1)[:, 0]
    e_p, e_idx = lax.top_k(jax.nn.softmax(e_logits, axis=-1), TOP_K)
    e_p = e_p / jnp.sum(e_p, axis=-1, keepdims=True)
    weights = g_p * e_p
    expert = g_idx * EXPERTS_PER_GROUP + e_idx
    NK = N * TOP_K
    flat_e = expert.reshape(NK)
    flat_tok = jnp.repeat(jnp.arange(N, dtype=jnp.int32), TOP_K)
    flat_w = weights.reshape(NK)
    order = jnp.argsort(flat_e)
    sorted_e = flat_e[order]
    counts = jnp.bincount(flat_e, length=N_EXPERTS)
    padded = (counts + MOE_BLOCK - 1) // MOE_BLOCK * MOE_BLOCK
    starts = jnp.cumsum(counts) - counts
    pad_ends = jnp.cumsum(padded)
    pad_starts = pad_ends - padded
    dest = pad_starts[sorted_e] + (jnp.arange(NK) - starts[sorted_e])
    cap = (NK + N_EXPERTS * (MOE_BLOCK - 1) + MOE_BLOCK - 1) // MOE_BLOCK * MOE_BLOCK
    n_blocks = cap // MOE_BLOCK
    row_tok = jnp.full((cap,), N, jnp.int32).at[dest].set(flat_tok[order])
    row_w = jnp.zeros((cap,), jnp.float32).at[dest].set(flat_w[order])
    block_e = jnp.minimum(jnp.searchsorted(pad_ends, jnp.arange(n_blocks) * MOE_BLOCK, side='right'),
                          N_EXPERTS - 1)
    t_pad = jnp.concatenate([t, jnp.zeros((1, D), t.dtype)], axis=0)

    def expert_block(args):
        rows, e = args
        xb = t_pad[rows]
        hid = jax.nn.silu(xb @ w_gate[e]) * (xb @ w_up[e])
        return hid @ w_down[e]

    y_rows = lax.map(expert_block, (row_tok.reshape(n_blocks, MOE_BLOCK), block_e))
    y_rows = y_rows.reshape(cap, D).astype(jnp.float32) * row_w[:, None]
    y = jnp.zeros((N + 1, D), jnp.float32).at[row_tok].add(y_rows)[:N]
    return y.reshape(B, S, D).astype(h.dtype)


def hybrid_layer(x, c, positions, cos, sin, w_ada, b_ada, norm1_g, w_in, q_a_norm, w_q_b, kv_a_norm,
                 w_kv_b, q_norm, k_norm, conv_w, conv_b, lru_wa, lru_ba, lru_wx, lru_bx, lru_lambda,
                 attn_out_norm, lru_out_norm, w_out, norm2_g, w_router_group, b_router_group,
                 w_router_expert, b_router_expert, w_gate, w_up, w_down):
    mod = jax.nn.silu(c) @ w_ada + b_ada
    shift1, scale1, gate1, shift2, scale2, gate2 = jnp.split(mod[:, None, :], 6, axis=-1)
    h = rms_norm(x, norm1_g) * (1.0 + scale1) + shift1
    proj = h @ w_in
    q_lat, kv_lat, k_rope, x_lru, g_lru = jnp.split(proj, IN_SPLITS, axis=-1)
    attn = mla_attention(q_lat, kv_lat, k_rope, cos, sin, q_a_norm, w_q_b, kv_a_norm, w_kv_b,
                         q_norm, k_norm)
    lru = rg_lru_branch(x_lru, g_lru, positions, conv_w, conv_b, lru_wa, lru_ba, lru_wx, lru_bx,
                        lru_lambda)
    mixed = jnp.concatenate([rms_norm(attn, attn_out_norm), rms_norm(lru, lru_out_norm)], axis=-1)
    x = x + gate1 * (mixed @ w_out)
    h2 = rms_norm(x, norm2_g) * (1.0 + scale2) + shift2
    x = x + gate2 * hierarchical_moe(h2, w_router_group, b_router_group, w_router_expert,
                                     b_router_expert, w_gate, w_up, w_down)
    return x


def setup_inputs(seed: int = 0) -> dict:
    key = jax.random.key(seed)
    ks = iter(jax.random.split(key, 40))
    L = DEPTH

    def normal(shape, scale):
        return jax.random.normal(next(ks), shape, jnp.float32) * scale

    def gain(n):
        return 1.0 + normal((L, n), 0.02)

    u = jax.random.uniform(next(ks), (L, LRU_WIDTH), jnp.float32, 0.9, 0.999)
    s = u ** (1.0 / LRU_C)
    lam = jnp.log(s) - jnp.log1p(-s)
    return {
        'x': normal((BATCH, SEQ, D_MODEL), 1.0),
        'c': normal((BATCH, D_MODEL), 1.0),
        'positions': jnp.tile(jnp.arange(SEQ, dtype=jnp.int32)[None, :], (BATCH, 1)),
        'w_ada': normal((L, D_MODEL, 6 * D_MODEL), D_MODEL ** -0.5),
        'b_ada': normal((L, 6 * D_MODEL), 0.02),
        'norm1_g': gain(D_MODEL),
        'w_in': normal((L, D_MODEL, IN_PROJ_WIDTH), D_MODEL ** -0.5),
        'q_a_norm': gain(Q_LORA_RANK),
        'w_q_b': normal((L, Q_LORA_RANK, ATTN_HEADS * QK_HEAD_DIM), Q_LORA_RANK ** -0.5),
        'kv_a_norm': gain(KV_LORA_RANK),
        'w_kv_b': normal((L, KV_LORA_RANK, ATTN_HEADS * (QK_NOPE_DIM + V_HEAD_DIM)), KV_LORA_RANK ** -0.5),
        'q_norm': gain(QK_HEAD_DIM),
        'k_norm': gain(QK_HEAD_DIM),
        'conv_w': normal((L, CONV_WIDTH, LRU_WIDTH), CONV_WIDTH ** -0.5),
        'conv_b': normal((L, LRU_WIDTH), 0.02),
        'lru_wa': normal((L, LRU_HEADS, LRU_BLOCK, LRU_BLOCK), LRU_BLOCK ** -0.5),
        'lru_ba': normal((L, LRU_WIDTH), 0.02),
        'lru_wx': normal((L, LRU_HEADS, LRU_BLOCK, LRU_BLOCK), LRU_BLOCK ** -0.5),
        'lru_bx': normal((L, LRU_WIDTH), 0.02),
        'lru_lambda': lam,
        'attn_out_norm': gain(ATTN_WIDTH),
        'lru_out_norm': gain(LRU_WIDTH),
        'w_out': normal((L, D_MIX, D_MODEL), D_MIX ** -0.5),
        'norm2_g': gain(D_MODEL),
        'w_router_group': normal((L, D_MODEL, N_GROUPS), D_MODEL ** -0.5),
        'b_router_group': normal((L, N_GROUPS), 0.01),
        'w_router_expert': normal((L, D_MODEL, N_EXPERTS), D_MODEL ** -0.5),
        'b_router_expert': normal((L, N_EXPERTS), 0.01),
        'w_gate': normal((L, N_EXPERTS, D_MODEL, D_FF_EXPERT), D_MODEL ** -0.5),
        'w_up': normal((L, N_EXPERTS, D_MODEL, D_FF_EXPERT), D_MODEL ** -0.5),
        'w_down': normal((L, N_EXPERTS, D_FF_EXPERT, D_MODEL), D_FF_EXPERT ** -0.5),
    }


def reference(x, c, positions, w_ada, b_ada, norm1_g, w_in, q_a_norm, w_q_b, kv_a_norm, w_kv_b,
              q_norm, k_norm, conv_w, conv_b, lru_wa, lru_ba, lru_wx, lru_bx, lru_lambda,
              attn_out_norm, lru_out_norm, w_out, norm2_g, w_router_group, b_router_group,
              w_router_expert, b_router_expert, w_gate, w_up, w_down):
    cos, sin = rope_cos_sin(positions)
    for l in range(DEPTH):
        x = hybrid_layer(x, c, positions, cos, sin, w_ada[l], b_ada[l], norm1_g[l], w_in[l],
                         q_a_norm[l], w_q_b[l], kv_a_norm[l], w_kv_b[l], q_norm[l], k_norm[l],
                         conv_w[l], conv_b[l], lru_wa[l], lru_ba[l], lru_wx[l], lru_bx[l],
                         lru_lambda[l], attn_out_norm[l], lru_out_norm[l], w_out[l], norm2_g[l],
                         w_router_group[l], b_router_group[l], w_router_expert[l],
                         b_router_expert[l], w_gate[l], w_up[l], w_down[l])
    return x
```

```python
import numpy as np
import concourse.bass as bass
import concourse.mybir as mybir
from contextlib import ExitStack

F32 = mybir.dt.float32
BF16 = mybir.dt.bfloat16
I32 = mybir.dt.int32
U32 = mybir.dt.uint32
U8 = mybir.dt.uint8
AF = mybir.ActivationFunctionType
ALU = mybir.AluOpType
AX = mybir.AxisListType

DT_SIZE = {F32: 4, BF16: 2, I32: 4, U32: 4, U8: 1}


class V:
    __slots__ = ("ap", "key")

    def __init__(self, ap, key):
        self.ap = ap
        self.key = key

    def __getitem__(self, idx):
        return V(self.ap[idx], self.key)

    def k(self, tag):
        base = self.key[0] if isinstance(self.key, tuple) else self.key
        return V(self.ap, (base, tag))

    def ks(self, tags):
        base = self.key[0] if isinstance(self.key, tuple) else self.key
        return V(self.ap, [(base, t) for t in tags])

    def bc(self, shape):
        return V(self.ap.to_broadcast(list(shape)), self.key)

    def re(self, s, **kw):
        return V(self.ap.rearrange(s, **kw), self.key)

    def bitcast(self, dt):
        return V(self.ap.bitcast(dt), self.key)

    @property
    def shape(self):
        return self.ap.shape


def _unwrap(x):
    return x.ap if isinstance(x, V) else x


class DmaGroup:
    def __init__(self, sem):
        self.sem = sem
        self.n = 0


class Sched:
    ENGS = ["pe", "act", "dve", "pool", "sp"]

    def __init__(self, nc, es, arena_bytes=204800, n_dma_sems=33):
        self.nc = nc
        self.es = es
        self.sem = {e: es.enter_context(nc.semaphore(f"sem_{e}")) for e in self.ENGS}
        self.cnt = {e: 0 for e in self.ENGS}
        self.ops = {e: [] for e in self.ENGS}
        self.lastw = {}
        self.readers = {}
        self.obs = {e: {} for e in self.ENGS}
        self.dma_sems = [es.enter_context(nc.semaphore(f"sem_dma{i}")) for i in range(n_dma_sems)]
        self.dma_sem_cnt = [0] * n_dma_sems
        self.dma_next = 0
        self.dma_next_pool = 0
        self.dma_next_sp = 0
        self.rr_n = n_dma_sems
        self.semobj = {}
        for e in self.ENGS:
            self.semobj[("eng", e)] = self.sem[e]
        for i, s in enumerate(self.dma_sems):
            self.semobj[("dma", i)] = s
        self.groups = []
        self.all_tokens = []
        self.arena = es.enter_context(nc.sbuf_tensor("arena", [128, arena_bytes], U8))
        self.arena_bytes = arena_bytes
        self.top = 0
        self.ps = [es.enter_context(nc.psum_tensor(f"psb{i}", [128, 512], F32)) for i in range(8)]

    def alloc(self, name, free_shape, dtype, parts=128):
        n = int(np.prod(free_shape)) * DT_SIZE[dtype]
        n_al = (n + 63) // 64 * 64
        assert self.top + n_al <= self.arena_bytes, f"arena overflow {name}: {self.top}+{n_al}"
        ap = self.arena[0:parts, self.top:self.top + n].bitcast(dtype)
        if len(free_shape) == 2:
            ap = ap.rearrange("p (a b) -> p a b", a=free_shape[0])
        elif len(free_shape) == 3:
            ap = ap.rearrange("p (a b c) -> p a b c", a=free_shape[0], b=free_shape[1])
        self.top += n_al
        return V(ap, name)

    def alloc_top(self, name, free_shape, dtype):
        n = int(np.prod(free_shape)) * DT_SIZE[dtype]
        n_al = (n + 63) // 64 * 64
        self.arena_bytes -= n_al
        assert self.top <= self.arena_bytes, f"arena overflow (top) {name}"
        ap = self.arena[:, self.arena_bytes:self.arena_bytes + n].bitcast(dtype)
        if len(free_shape) == 2:
            ap = ap.rearrange("p (a b) -> p a b", a=free_shape[0])
        return V(ap, name)

    def free_top(self, nbytes):
        self.barrier()
        self.arena_bytes += (nbytes + 63) // 64 * 64

    def mark(self):
        return self.top

    def release(self, m):
        self.barrier()
        self.top = m

    def psum(self, bank, dtype=F32):
        ap = self.ps[bank][:, :]
        if dtype != F32:
            ap = ap.bitcast(dtype)
        return V(ap, f"ps{bank}")

    def _deps(self, eng, reads, writes):
        waits = {}

        def need(tok, raw=False):
            if tok is None:
                return
            sk, val = tok
            if sk == ("eng", "pe") and eng == "pe":
                return
            if sk == ("eng", eng) and not raw:
                return
            if self.obs[eng].get(sk, -1) >= val:
                return
            if waits.get(sk, -1) < val:
                waits[sk] = val

        for k in reads:
            need(self.lastw.get(k), raw=True)
        for k in writes:
            need(self.lastw.get(k))
            for t in self.readers.get(k, ()):
                need(t)
        for sk, val in waits.items():
            self.obs[eng][sk] = val
        return list(waits.items())

    def _commit(self, tok, reads, writes):
        for k in writes:
            self.lastw[k] = tok
            self.readers[k] = []
        for k in reads:
            if k in writes:
                continue
            self.readers.setdefault(k, []).append(tok)

    def op(self, eng, fn, reads, writes):
        reads = [k for k in reads if k is not None]
        writes = [k for k in writes if k is not None]
        writes = writes + [k for k in reads if isinstance(k, str) and k.startswith("ps") and k not in writes]
        waits = self._deps(eng, reads, writes)
        self.cnt[eng] += 1
        tok = (("eng", eng), self.cnt[eng])
        self.ops[eng].append((waits, fn, ("eng", eng), 1))
        self._commit(tok, reads, writes)
        return tok

    def dma(self, eng, out, in_, group=None, **kw):
        reads, writes = self._rw([out], [in_])
        waits = self._deps(eng, reads, writes)
        o, i = _unwrap(out), _unwrap(in_)
        if group is not None:
            group.n += 1
            sk = group.sem
            tok = (sk, float("inf"))
        else:
            idx = self._next_dma_sem(eng)
            sk = ("dma", idx)
            prev = self.dma_sem_cnt[idx]
            if prev > 0 and self.obs[eng].get(sk, -1) < prev:
                waits.append((sk, prev))
                self.obs[eng][sk] = prev
            self.dma_sem_cnt[idx] = prev + 16
            tok = (sk, prev + 16)
        qeng = eng

        def fn(e, o=o, i=i, kw=kw):
            return e.dma_start(out=o, in_=i, **kw)

        self.cnt[eng] += 0
        self.ops[qeng].append((waits, fn, sk, 16))
        self._commit(tok, reads, writes)
        self.all_tokens.append(tok)
        return tok

    def dma_custom(self, eng, fn, reads, writes):
        waits = self._deps(eng, list(reads), list(writes))
        idx = self._next_dma_sem(eng)
        sk = ("dma", idx)
        prev = self.dma_sem_cnt[idx]
        if prev > 0 and self.obs[eng].get(sk, -1) < prev:
            waits.append((sk, prev))
            self.obs[eng][sk] = prev
        self.dma_sem_cnt[idx] = prev + 16
        tok = (sk, prev + 16)
        self.ops[eng].append((waits, fn, sk, 16))
        self._commit(tok, list(reads), list(writes))
        self.all_tokens.append(tok)
        return tok

    def _next_dma_sem(self, eng):
        half = self.rr_n // 2
        if eng == "pool":
            i = self.dma_next_pool
            self.dma_next_pool = (i + 1) % half
            return i
        i = self.dma_next_sp
        self.dma_next_sp = (i + 1) % (self.rr_n - half)
        return half + i

    def new_group(self):
        idx = len(self.dma_sems) - 1 - len(self.groups)
        self.rr_n = idx
        assert self.dma_next < self.rr_n
        g = DmaGroup(("dma", idx))
        self.groups.append(g)
        return g

    def barrier(self):
        toks = [(("eng", e), self.cnt[e]) for e in self.ENGS if self.cnt[e] > 0]
        toks += self.all_tokens
        self.all_tokens = []
        for e in self.ENGS:
            waits = {}
            for sk, val in toks:
                if sk == ("eng", e) and e == "pe":
                    continue
                if self.obs[e].get(sk, -1) >= val:
                    continue
                if waits.get(sk, -1) < val:
                    waits[sk] = val
            for sk, val in waits.items():
                self.obs[e][sk] = val
            if waits:
                self.ops[e].append((list(waits.items()), None, None, 0))
        self.lastw = {}
        self.readers = {}

    def _rw(self, out_list, in_list):
        W, R = [], []
        for lst, dst in ((out_list, W), (in_list, R)):
            for x in lst:
                if isinstance(x, V):
                    if isinstance(x.key, list):
                        dst.extend(x.key)
                    else:
                        dst.append(x.key)
        return R, W

    def mm(self, out, lhsT, rhs, start=True, stop=True, extraR=(), **kw):
        R, W = self._rw([out], [lhsT, rhs])
        if not start:
            R = R + W
        o, l, r = _unwrap(out), _unwrap(lhsT), _unwrap(rhs)
        return self.op("pe", lambda e: e.matmul(o, l, r, start=start, stop=stop, **kw), R + list(extraR), W)

    def tr(self, out, in_, ident):
        R, W = self._rw([out], [in_, ident])
        o, i, d = _unwrap(out), _unwrap(in_), _unwrap(ident)
        return self.op("pe", lambda e: e.transpose(o, i, d), R, W)

    def act(self, out, in_, func, bias=None, scale=None, accum_out=None, eng="act"):
        outs = [out] + ([accum_out] if accum_out is not None else [])
        ins = [in_] + [x for x in (bias, scale) if isinstance(x, V)]
        R, W = self._rw(outs, ins)
        kw = {}
        if bias is not None:
            kw["bias"] = _unwrap(bias)
        if scale is not None:
            kw["scale"] = _unwrap(scale)
        if accum_out is not None:
            kw["accum_out"] = _unwrap(accum_out)
        o, i = _unwrap(out), _unwrap(in_)
        return self.op("act", lambda e: e.activation(o, i, func, **kw), R, W)

    def v(self, eng, method, outs, ins, *args, **kwargs):
        R, W = self._rw(outs, ins)
        a = [_unwrap(x) for x in args]
        k = {n: _unwrap(x) for n, x in kwargs.items()}
        return self.op(eng, lambda e: getattr(e, method)(*a, **k), R, W)

    def tt(self, out, in0, in1, op, eng="dve"):
        return self.v(eng, "tensor_tensor", [out], [in0, in1], out, in0, in1, op)

    def ts(self, out, in0, s1, s2, op0, op1=None, eng="dve", accum_out=None):
        ins = [in0] + [x for x in (s1, s2) if isinstance(x, V)]
        outs = [out] + ([accum_out] if accum_out is not None else [])
        kw = {}
        if op1 is not None:
            kw["op1"] = op1
        if accum_out is not None:
            kw["accum_out"] = accum_out
        return self.v(eng, "tensor_scalar", outs, ins, out, in0, s1, s2, op0, **kw)

    def stt(self, out, in0, scalar, in1, op0, op1, eng="dve"):
        ins = [in0, in1] + ([scalar] if isinstance(scalar, V) else [])
        return self.v(eng, "scalar_tensor_tensor", [out], ins, out, in0, scalar, in1, op0, op1)

    def copy(self, out, in_, eng="dve"):
        if eng == "act":
            return self.act(out, in_, AF.Copy)
        return self.v(eng, "tensor_copy", [out], [in_], out, in_)

    def memset(self, out, val, eng="dve"):
        return self.v(eng, "memset", [out], [], out, val)

    def recip(self, out, in_, eng="dve"):
        return self.v(eng, "reciprocal", [out], [in_], out, in_)

    def emit(self):
        nc = self.nc
        engmap = {"pe": "tensor", "act": "scalar", "dve": "vector", "pool": "gpsimd", "sp": "sync"}
        self.barrier()
        sched = self

        def resolve(sk, val):
            if val == float("inf"):
                for g in sched.groups:
                    if g.sem == sk:
                        return 16 * g.n
                raise RuntimeError("group not found")
            return val

        with nc.Block() as block:
            for ename in self.ENGS:
                ops = self.ops[ename]

                def body(e, ops=ops):
                    for waits, fn, incsem, incval in ops:
                        for sk, val in waits:
                            e.wait_ge(sched.semobj[sk], resolve(sk, val))
                        if fn is not None:
                            ins = fn(e)
                            ins.then_inc(sched.semobj[incsem], incval)

                getattr(block, engmap[ename])(body)
from concourse.bass_utils import run_bass_kernel_spmd
import math
import os

C_CT, C_BADA, C_G1, C_G2, C_KNG, C_CW, C_CB, C_BA, C_BX, C_LAM, C_GA, C_GL, C_PE, C_PO = \
    0, 8, 56, 64, 72, 73, 89, 93, 97, 101, 105, 109, 113, 114
C_PCOL = 115
NS = 116
R_GQA, R_GKVA, R_GQ, R_GKPE, R_INV, R_BR = 0, 256, 384, 576, 640, 672
R_J = 708
NR = 804
EPS = 1e-6
TWO_PI = 2.0 * math.pi
PI_LO = 3.1415925


def build_nc(stop_after=99, debug=False):
    nc = bass.Bass("TRN2", target_bir_lowering=False)

    def din(name, shape, dt=F32):
        return nc.dram_tensor(name, list(shape), dt, kind="ExternalInput").ap()

    xf_d = din("xf", [8192, 1024])
    xo_d = din("xo", [4096, 1024])
    posf_d = din("posf_pm", [128, 64], I32)
    poso_d = din("poso_pm", [128, 32], I32)
    posrow_d = din("posf_row", [1, 8192], I32)
    sm_d = din("smalls", [128, NS])
    rows_d = din("rows", [128, NR])
    ident_d = din("ident", [128, 128])
    mask_d = din("maskf", [128, 8, 512])
    wada_d = din("w_ada", [1024, 6144])
    win_d = din("w_in", [1024, 1472])
    wqb_d = din("w_q_b", [256, 768])
    wkvb_d = din("w_kv_b", [128, 1024])
    wa_d = din("lru_wa", [4, 128, 128])
    wx_d = din("lru_wx", [4, 128, 128])
    wout_d = din("w_out", [1024, 1024])
    wr_d = din("w_router", [1024, 36])
    wgl_d = din("wgu_lo", [4096, 2048])
    wgh_d = din("wgu_hi", [4096, 2048])
    wdl_d = din("wd_l", [4096, 2048])
    ltri_d = din("ltri", [128, 128])
    X1_d = nc.dram_tensor("X1_scr", [4096, 1024], F32, kind="Internal").ap()
    H2_d = nc.dram_tensor("H2_scr", [4096, 1024], BF16, kind="Internal").ap()
    Xg_d = nc.dram_tensor("Xg_scr", [96 * 128, 1024], BF16, kind="Internal").ap()
    Y_d = nc.dram_tensor("Y_scr", [96 * 128, 1024], F32, kind="Internal").ap()
    wb_d = [nc.dram_tensor(f"wb_scr{i}", [4096, 2048], BF16, kind="Internal").ap() for i in range(3)]
    wsrc_d = [wgl_d, wgh_d, wdl_d]
    wb_keys = []
    out_d = nc.dram_tensor("out", [4096, 1024], F32, kind="ExternalOutput").ap()
    dbg = {}

    def dout(name, shape, dt=F32):
        dbg[name] = nc.dram_tensor(name, list(shape), dt, kind="ExternalOutput").ap()
        return dbg[name]

    with ExitStack() as es:
        S = Sched(nc, es, arena_bytes=212480)
        G = S.new_group()
        sm = S.alloc("sm", [NS], F32)
        rows = S.alloc("rows", [NR], F32)
        identf = S.alloc("identf", [128], F32)
        identb = S.alloc("identb", [128], BF16)
        onesf = S.alloc("onesf", [128], F32)
        onesb = S.alloc("onesb", [128], BF16)
        modT = S.alloc("modT", [48], F32)
        a1 = S.alloc("a1", [8], F32)
        a2 = S.alloc("a2", [8], F32)
        sc = S.alloc("sc", [8], F32)
        cA = S.alloc("cA", [4], F32)
        cA2 = S.alloc("cA2", [4], F32)
        hown = S.alloc("hown", [4, 4096], BF16)
        markA = S.mark()
        kvnT = S.alloc("kvnT", [8192], BF16)
        RT = S.alloc("RT", [8192], BF16)
        sspe = S.alloc("sspe", [64], F32)
        coso = S.alloc("coso", [32, 32], F32)
        sino = S.alloc("sino", [32, 32], F32)
        S.dma("sp", sm, sm_d, group=G)
        S.dma("sp", rows, rows_d, group=G)
        S.dma("sp", identf, ident_d, group=G)
        S.copy(identb, identf)
        S.memset(onesf, 1.0)
        S.memset(onesb, 1.0, eng="pool")
        sh1 = modT[:, 0:8]
        sh2 = modT[:, 24:32]

        m0 = S.mark()
        cosf = S.alloc("cosf", [64, 32], F32)
        sinf = S.alloc("sinf", [64, 32], F32)
        m0b = S.mark()
        S.act(sc, sm[:, C_CT:C_CT + 8], AF.Silu)
        psA = S.psum(0)
        wada_v = wada_d.rearrange("(k p) n -> p k n", p=128)
        wt = [S.alloc(f"wada{i}", [8, 1024], F32) for i in range(2)]
        for g in range(2):
            t = wt[g % 2]
            S.dma("sp", t, wada_v[:, :, g * 1024:(g + 1) * 1024])
            for j in range(8):
                col = g * 8 + j
                for k in range(8):
                    S.mm(psA[:, col:col + 1], t[:, k, j * 128:(j + 1) * 128], sc[:, k:k + 1],
                         start=(k == 0), stop=(k == 7))
        S.tt(modT[:, 0:16], psA[:, 0:16], sm[:, C_BADA:C_BADA + 16], ALU.add)
        S.stt(a1, modT[:, 8:16], 1.0, sm[:, C_G1:C_G1 + 8], ALU.add, ALU.mult)
        e4 = S.alloc("e4", [4], F32)
        S.act(e4, sm[:, C_LAM:C_LAM + 4], AF.Exp, scale=-1.0)
        S.act(e4, e4, AF.Ln, bias=1.0)
        S.ts(cA, e4, -8.0, None, ALU.mult)
        S.ts(cA2, e4, -16.0, None, ALU.mult)

        def trig(pos_d, NT, cos_t, sin_t, tag):
            mm_ = S.mark()
            posi = S.alloc(f"posi{tag}", [NT], I32)
            posf = S.alloc(f"posf{tag}", [NT], F32)
            ang = S.alloc(f"ang{tag}", [NT, 32], F32)
            kf = S.alloc(f"kf{tag}", [NT, 32], F32)
            ki = S.alloc(f"ki{tag}", [NT, 32], I32)
            w = S.alloc(f"w{tag}", [NT, 32], F32)
            S.dma("sp", posi, pos_d)
            S.copy(posf, posi)
            pb = V(posf.ap.unsqueeze(2).to_broadcast([128, NT, 32]), posf.key)
            ib = V(rows[:, R_INV:R_INV + 32].ap.unsqueeze(1).to_broadcast([128, NT, 32]), rows.key)
            S.tt(ang, pb, ib, ALU.mult)
            S.ts(kf, ang, 1.0 / TWO_PI, None, ALU.mult)
            S.copy(ki, kf)
            S.copy(kf, ki)
            S.stt(ang, kf, -6.28125, ang, ALU.mult, ALU.add)
            S.stt(ang, kf, -(TWO_PI - 6.28125), ang, ALU.mult, ALU.add)

            def wrap(t):
                S.ts(w, t, math.pi, -TWO_PI, ALU.is_gt, ALU.mult)
                S.tt(t, t, w, ALU.add)
                S.ts(w, t, -math.pi, TWO_PI, ALU.is_lt, ALU.mult)
                S.tt(t, t, w, ALU.add)
                S.ts(t, t, PI_LO, -PI_LO, ALU.min, ALU.max)

            wrap(ang)
            S.act(sin_t, ang, AF.Sin)
            S.ts(ang, ang, math.pi / 2, None, ALU.add)
            wrap(ang)
            S.act(cos_t, ang, AF.Sin)
            S.release(mm_)

        trig(posf_d, 64, cosf, sinf, "f")
        trig(poso_d, 32, coso, sino, "o")
        if debug:
            S.dma("sp", dout("d_cosf", [128, 64, 32]), cosf)
            S.dma("sp", dout("d_sinf", [128, 64, 32]), sinf)
            S.dma("sp", dout("d_cA", [128, 4]), cA)
        S.release(m0b)
        if stop_after <= 0:
            S.emit()
            return nc, dbg

        win = S.alloc("win", [8, 704], BF16)
        S.dma("pool", win, win_d.rearrange("(k p) n -> p k n", p=128)[:, :, 256:960])
        wa = S.alloc("wa", [4, 128], BF16)
        wx = S.alloc("wx", [4, 128], BF16)
        S.dma("pool", wa, wa_d.rearrange("h i j -> i h j"))
        S.dma("pool", wx, wx_d.rearrange("h i j -> i h j"))
        xt = [S.alloc(f"xt{i}", [1024], F32) for i in range(4)]
        junk = S.alloc("junk", [1024], BF16)
        hT = S.alloc("hT", [8, 512], BF16)
        ss4 = S.alloc("ss4", [4], F32)
        rstd4 = S.alloc("rstd4", [4], F32)
        diag = [S.alloc(f"diag{i}", [128], F32) for i in range(2)]
        kvn = S.alloc("kvn", [4, 128], BF16)
        kst = S.alloc("kst", [8], F32)
        kpg = S.alloc("kpg", [4, 64], F32)
        Rtm = S.alloc("Rtm", [4, 64], BF16)
        rt1 = S.alloc("rt1", [4, 32], F32)
        rt2 = S.alloc("rt2", [4, 32], F32)
        xbuf = S.alloc("xbuf", [4, 515], F32)
        xc = S.alloc("xc", [4, 512], F32)
        xcb = S.alloc("xcb", [4, 512], BF16)
        rr = S.alloc("rr", [4, 512], F32)
        ii = S.alloc("ii", [4, 512], F32)
        mm_t = S.alloc("mm_t", [4, 512], F32)
        hh = S.alloc("hh", [4, 512], F32)
        carry = S.alloc("carry", [4], F32)
        posb = S.alloc("posb", [512], I32)
        keep = S.alloc("keep", [512], F32)
        seltmp = S.alloc("seltmp", [2, 128], F32)
        S.memset(xbuf, 0.0)
        S.memset(carry, 0.0)
        S.memset(RT[64:128, :], 0.0, eng="pool")
        pe_c = sm[:, C_PE:C_PE + 1]
        po_c = sm[:, C_PO:C_PO + 1]
        psT = [S.psum(1), S.psum(2)]
        psKV = S.psum(3)
        psL = [S.psum(4), S.psum(5), S.psum(6), S.psum(7)]
        hTs = [hT, S.alloc("hT_b", [8, 512], BF16)]

        def P1(st):
            hT = hTs[st % 2]
            for s in range(4):
                if s > 0:
                    yield
                ti = st * 4 + s
                xb = xt[ti % 4]
                S.dma("sp", xb, xf_d[ti * 128:(ti + 1) * 128, :])
                S.act(junk, xb, AF.Square, accum_out=ss4[:, s:s + 1])
                S.act(rstd4[:, s:s + 1], ss4[:, s:s + 1], AF.Ln, scale=1.0 / 1024, bias=EPS)
                S.act(rstd4[:, s:s + 1], rstd4[:, s:s + 1], AF.Exp, scale=-0.5)
                dg = diag[ti % 2]
                S.ts(dg, identf, rstd4[:, s:s + 1], None, ALU.mult)
                for half in range(2):
                    ps = psT[half]
                    for kk in range(4):
                        k = half * 4 + kk
                        S.mm(ps[:, kk * 128:(kk + 1) * 128], xb[:, k * 128:(k + 1) * 128], dg)
                    for kk in range(4):
                        k = half * 4 + kk
                        dst = hT[:, k, s * 128:(s + 1) * 128].k((k, s))
                        src = ps[:, kk * 128:(kk + 1) * 128]
                        if half == 0:
                            S.act(dst, src, AF.Identity, scale=a1[:, k:k + 1], bias=sh1[:, k:k + 1])
                        else:
                            S.ts(dst, src, a1[:, k:k + 1], sh1[:, k:k + 1], ALU.mult, ALU.add)
            yield

        def P2(st):
            hT = hTs[st % 2]
            for c in range(4):
                for k in range(8):
                    S.mm(psL[c], win[:, k, 192 + c * 128:192 + (c + 1) * 128], hT[:, k, :].ks([(k, q) for q in range(4)]),
                         start=(k == 0), stop=(k == 7))
            yield
            for s in range(4):
                if s > 0:
                    yield
                ti = st * 4 + s
                o = (s % 2) * 192
                for k in range(8):
                    S.mm(psKV[:, o:o + 192], hT[:, k, s * 128:(s + 1) * 128].k((k, s)), win[:, k, 0:192],
                         start=(k == 0), stop=(k == 7))
                S.act(junk[:, 0:128], psKV[:, o:o + 128], AF.Square, accum_out=kst[:, s:s + 1])
                S.act(kst[:, 4 + s:5 + s], kst[:, s:s + 1], AF.Ln, scale=1.0 / 128, bias=EPS)
                S.act(kst[:, 4 + s:5 + s], kst[:, 4 + s:5 + s], AF.Exp, scale=-0.5)
                S.stt(kvn[:, s, :], psKV[:, o:o + 128], kst[:, 4 + s:5 + s], rows[:, R_GKVA:R_GKVA + 128],
                      ALU.mult, ALU.mult)
                S.act(junk[:, 128:192], psKV[:, o + 128:o + 192], AF.Square, accum_out=sspe[:, ti:ti + 1])
                S.tt(kpg[:, s, :], psKV[:, o + 128:o + 192], rows[:, R_GKPE:R_GKPE + 64], ALU.mult)
            yield
            cs = cosf[:, st * 4:(st + 1) * 4, :]
            sn = sinf[:, st * 4:(st + 1) * 4, :]
            x1 = kpg[:, :, 0:32]
            x2 = kpg[:, :, 32:64]
            S.tt(rt1, x1, cs, ALU.mult)
            S.tt(rt2, x2, sn, ALU.mult)
            S.tt(Rtm[:, :, 0:32], rt1, rt2, ALU.subtract)
            S.tt(rt1, x2, cs, ALU.mult)
            S.tt(rt2, x1, sn, ALU.mult)
            S.tt(Rtm[:, :, 32:64], rt1, rt2, ALU.add)
            pst = S.psum(0, BF16)
            for s in range(4):
                S.tr(pst[:, s * 128:(s + 1) * 128], kvn[:, s, :], identb)
            S.copy(kvnT[:, st * 512:(st + 1) * 512], pst[:, 0:512], eng="act")
            pst2 = S.psum(0, BF16)
            for s in range(4):
                S.tr(pst2[0:64, s * 128:(s + 1) * 128], Rtm[:, s, :], identb)
            S.copy(RT[0:64, st * 512:(st + 1) * 512], pst2[0:64, 0:512])
            yield
            S.dma("sp", posb, posrow_d[:, st * 512:(st + 1) * 512].partition_broadcast(128))
            S.ts(keep, posb, 0.0, None, ALU.not_equal)
            for c in range(4):
                xb_c = xbuf[:, c, :].k(c)
                S.copy(xb_c[:, 3:515], psL[c], eng="act")
                cw = lambda j: sm[:, C_CW + c * 4 + j:C_CW + c * 4 + j + 1]
                S.ts(xc[:, c, :].k(c), xb_c[:, 3:515], cw(3), sm[:, C_CB + c:C_CB + c + 1], ALU.mult, ALU.add)
                for j in range(3):
                    S.stt(xc[:, c, :].k(c), xb_c[:, j:j + 512], cw(j), xc[:, c, :].k(c), ALU.mult, ALU.add)
                S.copy(xb_c[:, 0:3], xb_c[:, 512:515])
                S.copy(xcb[:, c, :].k(c), xc[:, c, :].k(c), eng="act")
                yield
            for c in range(4):
                S.mm(psL[c], wa[:, c, :], xcb[:, c, :].k(c))
            for c in range(4):
                S.act(rr[:, c, :].k(c), psL[c], AF.Sigmoid, bias=sm[:, C_BA + c:C_BA + c + 1])
            yield
            for c in range(4):
                S.mm(psL[c], wx[:, c, :], xcb[:, c, :].k(c))
            for c in range(4):
                S.act(ii[:, c, :].k(c), psL[c], AF.Sigmoid, bias=sm[:, C_BX + c:C_BX + c + 1])
            yield
            for c in range(4):
                S.act(mm_t[:, c, :].k(c), rr[:, c, :].k(c), AF.Exp, scale=cA2[:, c:c + 1])
                S.act(rr[:, c, :].k(c), rr[:, c, :].k(c), AF.Exp, scale=cA[:, c:c + 1])
            for c in range(4):
                S.ts(mm_t[:, c, :].k(c), mm_t[:, c, :].k(c), 0.9999999, None, ALU.min)
                S.act(mm_t[:, c, :].k(c), mm_t[:, c, :].k(c), AF.Ln, scale=-1.0, bias=1.0)
                S.act(mm_t[:, c, :].k(c), mm_t[:, c, :].k(c), AF.Exp, scale=0.5)
            yield
            for c in range(4):
                if c > 0:
                    yield
                S.tt(ii[:, c, :].k(c), ii[:, c, :].k(c), xc[:, c, :].k(c), ALU.mult)
                S.tt(rr[:, c, :].k(c), rr[:, c, :].k(c), keep, ALU.mult)
                S.stt(mm_t[:, c, :].k(c), mm_t[:, c, :].k(c), -1.0, keep, ALU.add, ALU.mult)
                S.stt(ii[:, c, :].k(c), mm_t[:, c, :].k(c), 1.0, ii[:, c, :].k(c), ALU.add, ALU.mult)
                S.v("dve", "tensor_tensor_scan", [hh[:, c, :].k(c)], [rr[:, c, :].k(c), ii[:, c, :].k(c), carry[:, c:c + 1]],
                    hh[:, c, :].k(c), rr[:, c, :].k(c), ii[:, c, :].k(c), carry[:, c:c + 1], ALU.mult, ALU.add)
                S.copy(carry[:, c:c + 1], hh[:, c, 511:512].k(c))
                hv = V(hh[:, c, :].ap.rearrange("p (j q t) -> p j q t", j=2, q=2), ("hh", c))
                S.ts(seltmp, hv[:, :, 1, :], po_c, None, ALU.mult)
                dst = V(hown[:, c, st * 256:(st + 1) * 256].ap.rearrange("p (j t) -> p j t", j=2), hown.key)
                S.stt(dst, hv[:, :, 0, :], pe_c, seltmp, ALU.mult, ALU.add)

            yield

        def interleave(ga, gb, ratio):
            da = db = False
            while not (da and db):
                for _ in range(ratio):
                    if not da:
                        try:
                            next(ga)
                        except StopIteration:
                            da = True
                if not db:
                    try:
                        next(gb)
                    except StopIteration:
                        db = True

        for _ in P1(0):
            pass
        for st in range(16):
            if st + 1 < 16:
                interleave(P2(st), P1(st + 1), 1)
            else:
                for _ in P2(st):
                    pass
        if debug:
            S.dma("sp", dout("d_kvnT", [128, 8192], BF16), kvnT)
            S.dma("sp", dout("d_RT", [64, 8192], BF16), RT[0:64, :])
            S.dma("sp", dout("d_hown", [128, 4, 4096], BF16), hown)
            S.dma("sp", dout("d_sspe", [128, 64]), sspe)
        S.release(m0)
        if stop_after <= 1:
            S.emit()
            return nc, dbg

        attnT = S.alloc_top("attnT", [4, 4096], BF16)
        qnT = S.alloc_top("qnT", [2, 4096], BF16)
        m2 = S.mark()
        winq = S.alloc("winq", [8, 256], BF16)
        wing = S.alloc("wing", [8, 512], BF16)
        winv = win_d.rearrange("(k p) n -> p k n", p=128)
        S.dma("pool", winq, winv[:, :, 0:256])
        S.dma("pool", wing, winv[:, :, 960:1472])
        xt = [S.alloc(f"xt{i}", [1024], F32) for i in range(4)]
        junk = S.alloc("junk", [1024], BF16)
        hTs2 = [S.alloc(f"hT2_{i}", [8, 512], BF16) for i in range(2)]
        ss4 = S.alloc("ss4", [4], F32)
        rstd4 = S.alloc("rstd4", [4], F32)
        diag = [S.alloc(f"diag{i}", [128], F32) for i in range(2)]
        qst = S.alloc("qst", [8], F32)
        qnb = S.alloc("qnb", [4, 256], BF16)
        gxs = [S.alloc(f"gx{i}", [512], F32) for i in range(2)]
        gus = [S.alloc(f"gu{i}", [512], F32) for i in range(2)]
        wadab = S.alloc("wadab", [8, 512], F32)
        psT = [S.psum(1), S.psum(2)]
        psQ = S.psum(3)
        psL = [S.psum(4), S.psum(5), S.psum(6), S.psum(7)]
        psA0 = S.psum(0)
        GC = 2.0 * math.sqrt(2.0 / math.pi)

        def P1b(st):
            hT = hTs2[st % 2]
            for s_ in range(4):
                if s_ > 0:
                    yield
                ti = st * 4 + s_
                xb = xt[ti % len(xt)]
                S.dma("sp", xb, xo_d[ti * 128:(ti + 1) * 128, :])
                S.act(junk, xb, AF.Square, accum_out=ss4[:, s_:s_ + 1])
                S.act(rstd4[:, s_:s_ + 1], ss4[:, s_:s_ + 1], AF.Ln, scale=1.0 / 1024, bias=EPS)
                S.act(rstd4[:, s_:s_ + 1], rstd4[:, s_:s_ + 1], AF.Exp, scale=-0.5)
                dg = diag[ti % 2]
                S.ts(dg, identf, rstd4[:, s_:s_ + 1], None, ALU.mult)
                for half in range(2):
                    ps = psT[half]
                    for kk in range(4):
                        k = half * 4 + kk
                        S.mm(ps[:, kk * 128:(kk + 1) * 128], xb[:, k * 128:(k + 1) * 128], dg)
                    for kk in range(4):
                        k = half * 4 + kk
                        dst = hT[:, k, s_ * 128:(s_ + 1) * 128].k((k, s_))
                        src = ps[:, kk * 128:(kk + 1) * 128]
                        if half == 0:
                            S.act(dst, src, AF.Identity, scale=a1[:, k:k + 1], bias=sh1[:, k:k + 1])
                        else:
                            S.ts(dst, src, a1[:, k:k + 1], sh1[:, k:k + 1], ALU.mult, ALU.add)
            yield

        def P2b(st):
            hT = hTs2[st % 2]
            for c in range(4):
                for k in range(8):
                    S.mm(psL[c], wing[:, k, c * 128:(c + 1) * 128], hT[:, k, :].ks([(k, q) for q in range(4)]),
                         start=(k == 0), stop=(k == 7))
            yield
            for s_ in range(4):
                ti = st * 4 + s_
                o = (s_ % 2) * 256
                for k in range(8):
                    S.mm(psQ[:, o:o + 256], hT[:, k, s_ * 128:(s_ + 1) * 128].k((k, s_)), winq[:, k, :],
                         start=(k == 0), stop=(k == 7))
                S.act(junk[:, 0:256], psQ[:, o:o + 256], AF.Square, accum_out=qst[:, s_:s_ + 1])
                S.act(qst[:, 4 + s_:5 + s_], qst[:, s_:s_ + 1], AF.Ln, scale=1.0 / 256, bias=EPS)
                S.act(qst[:, 4 + s_:5 + s_], qst[:, 4 + s_:5 + s_], AF.Exp, scale=-0.5)
                S.stt(qnb[:, s_, :], psQ[:, o:o + 256], qst[:, 4 + s_:5 + s_], rows[:, R_GQA:R_GQA + 256],
                      ALU.mult, ALU.mult)
                yield
            for c in range(2):
                pst = S.psum(0, BF16)
                for s_ in range(4):
                    S.tr(pst[:, s_ * 128:(s_ + 1) * 128], qnb[:, s_, c * 128:(c + 1) * 128], identb)
                S.copy(qnT[:, c, st * 512:(st + 1) * 512], pst[:, 0:512], eng=("act" if c == 0 else "dve"))
            yield
            for c in range(4):
                gx, gu = gxs[c % 2], gus[c % 2]
                S.act(gx, psL[c], AF.Copy)
                S.act(gu, psL[c], AF.Square)
                S.ts(gu, gu, 0.044715, 1.0, ALU.mult, ALU.add)
                S.tt(gu, gu, gx, ALU.mult)
                S.act(gu, gu, AF.Sigmoid, scale=GC)
                S.tt(gu, gu, gx, ALU.mult)
                S.tt(hown[:, c, st * 512:(st + 1) * 512], hown[:, c, st * 512:(st + 1) * 512], gu, ALU.mult)
                yield

        def ADA(hg):
            g, hh_ = 2 + hg // 2, hg % 2
            S.dma("sp", wadab, wada_v[:, :, g * 1024 + hh_ * 512:g * 1024 + (hh_ + 1) * 512])
            yield
            for j4 in range(4):
                col = 300 + g * 8 + hh_ * 4 + j4
                for k in range(8):
                    S.mm(psA0[:, col:col + 1], wadab[:, k, j4 * 128:(j4 + 1) * 128], sc[:, k:k + 1],
                         start=(k == 0), stop=(k == 7))
                yield
            c0_ = g * 8 + hh_ * 4
            S.tt(modT[:, c0_:c0_ + 4], psA0[:, 300 + c0_:300 + c0_ + 4], sm[:, C_BADA + c0_:C_BADA + c0_ + 4], ALU.add)
            yield

        def interleave3(ga, gb, gc):
            gens = [ga, gb, gc]
            done = [g is None for g in gens]
            pattern = [0, 0, 1, 2]
            while not all(done):
                for gi in pattern:
                    if not done[gi]:
                        try:
                            next(gens[gi])
                        except StopIteration:
                            done[gi] = True

        for _ in P1b(0):
            pass
        for st in range(8):
            interleave3(P2b(st), P1b(st + 1) if st + 1 < 8 else None, ADA(st))
        if debug:
            S.dma("sp", dout("d_qnT", [128, 2, 4096], BF16), qnT)
            S.dma("sp", dout("d_lru", [128, 4, 4096], BF16), hown)
        S.release(m2)
        if stop_after <= 2:
            S.emit()
            return nc, dbg

        m3 = S.mark()
        wkvb = S.alloc("wkvb", [1024], BF16)
        wqb = S.alloc("wqb", [2, 768], BF16)
        S.dma("pool", wkvb, wkvb_d)
        S.dma("pool", wqb, wqb_d.rearrange("(c p) n -> p c n", p=128))
        maskb = S.alloc("maskb", [8, 512], BF16)
        for r_ in range(8):
            S.dma("pool", maskb[:, r_, :], mask_d[:, r_, :])
        KhT = S.alloc("KhT", [8192], BF16)
        Vh = S.alloc("Vh", [64, 128], BF16)
        QnT = S.alloc("QnT", [4096], BF16)
        QpT = S.alloc("QpT", [4096], BF16)
        ssk = S.alloc("ssk", [64], F32)
        sk = S.alloc("sk", [64], F32)
        junk = S.alloc("junk", [256], BF16)
        qst = S.alloc("qst", [8], F32)
        qg = S.alloc("qg", [4, 192], F32)
        qb16 = S.alloc("qb16", [4, 192], BF16)
        rt1 = S.alloc("rt1", [4, 32], F32)
        rt2 = S.alloc("rt2", [4, 32], F32)
        S.memset(QpT[64:128, :], 0.0, eng="pool")
        wstage = S.alloc("wstage", [2048], BF16)
        bg = {"it": 0}

        def bg_convert():
            it = bg["it"]
            bg["it"] += 1
            c = it // 12
            if c >= 96:
                return
            a_, e_ = c % 3, c // 3
            if it % 12 == 0:
                S.dma("pool", wstage, wsrc_d[a_][e_ * 128:(e_ + 1) * 128, :])
            elif it % 12 == 6:
                key = ("WB", c)
                wb_keys.append(key)
                S.dma("sp", V(wb_d[a_][e_ * 128:(e_ + 1) * 128, :], key), wstage)

        PT = [S.alloc(f"PT{i}", [512], BF16) for i in range(4)]
        den = S.alloc("den", [512], F32)
        rec = S.alloc("rec", [512], F32)
        dsum = S.alloc("dsum", [512], F32)
        SCL = 192.0 ** -0.5
        P3H = int(os.environ.get("P3H", "4"))
        P3T = int(os.environ.get("P3T", "8"))
        P3M = int(os.environ.get("P3M", "7"))
        for h in range(P3H):
            qgf = V(qg.ap.rearrange("p a b -> p (a b)"), qg.key)
            for i2 in range(32 if (P3M & 1) else 0):
                ps = S.psum(3 + (i2 % 4))
                for u in range(2):
                    i = i2 * 2 + u
                    S.mm(ps[:, u * 256:(u + 1) * 256], kvnT[:, i * 128:(i + 1) * 128], wkvb[:, h * 256:(h + 1) * 256])
                ps3 = V(ps.ap.rearrange("p (u c) -> p u c", u=2), ps.key)
                sqv = V(qgf[:, (i2 % 2) * 256:(i2 % 2) * 256 + 256].ap.rearrange("p (u c) -> p u c", u=2), qg.key)
                S.act(sqv, ps3[:, :, 0:128], AF.Square)
                S.act(Vh[:, i2 * 2:i2 * 2 + 2, :], ps3[:, :, 128:256], AF.Copy)
                S.v("dve", "tensor_reduce", [ssk], [sqv], ssk[:, i2 * 2:i2 * 2 + 2], sqv, AX.X, ALU.add)
            for n in range(16 if (P3M & 1) else 0):
                ps = S.psum(5 + (n % 2))
                S.mm(ps, wkvb[:, h * 256:h * 256 + 128], kvnT[:, n * 512:(n + 1) * 512])
                if n % 2 == 0:
                    S.act(KhT[:, n * 512:(n + 1) * 512], ps, AF.Identity, scale=sm[:, C_KNG:C_KNG + 1])
                else:
                    S.ts(KhT[:, n * 512:(n + 1) * 512], ps, sm[:, C_KNG:C_KNG + 1], None, ALU.mult)
            S.tt(sk, ssk, sspe, ALU.add)
            S.act(sk, sk, AF.Ln, scale=1.0 / 192, bias=EPS)
            S.act(sk, sk, AF.Exp, scale=-0.5)
            S.ts(sk, sk, SCL, None, ALU.mult)
            for st in range(8 if (P3M & 2) else 0):
                for s_ in range(4):
                    ti = st * 4 + s_
                    ps = S.psum(7 if s_ < 2 else 4)
                    o = (s_ % 2) * 192
                    for c in range(2):
                        S.mm(ps[:, o:o + 192], qnT[:, c, ti * 128:(ti + 1) * 128], wqb[:, c, h * 192:(h + 1) * 192],
                             start=(c == 0), stop=(c == 1))
                for s_ in range(4):
                    ps = S.psum(7 if s_ < 2 else 4)
                    o = (s_ % 2) * 192
                    S.act(junk[:, 0:192], ps[:, o:o + 192], AF.Square, accum_out=qst[:, s_:s_ + 1])
                S.act(qst[:, 4:8], qst[:, 0:4], AF.Ln, scale=1.0 / 192, bias=EPS)
                S.act(qst[:, 4:8], qst[:, 4:8], AF.Exp, scale=-0.5)
                for s_ in range(4):
                    ps = S.psum(7 if s_ < 2 else 4)
                    o = (s_ % 2) * 192
                    S.stt(qg[:, s_, :], ps[:, o:o + 192], qst[:, 4 + s_:5 + s_], rows[:, R_GQ:R_GQ + 192],
                          ALU.mult, ALU.mult)
                cs = coso[:, st * 4:(st + 1) * 4, :]
                sn = sino[:, st * 4:(st + 1) * 4, :]
                x1 = qg[:, :, 128:160]
                x2 = qg[:, :, 160:192]
                S.copy(qb16[:, :, 0:128], qg[:, :, 0:128])
                S.tt(rt1, x1, cs, ALU.mult)
                S.tt(rt2, x2, sn, ALU.mult)
                S.tt(qb16[:, :, 128:160], rt1, rt2, ALU.subtract)
                S.tt(rt1, x2, cs, ALU.mult)
                S.tt(rt2, x1, sn, ALU.mult)
                S.tt(qb16[:, :, 160:192], rt1, rt2, ALU.add)
                pst = S.psum(5, BF16)
                for s_ in range(4):
                    S.tr(pst[:, s_ * 128:(s_ + 1) * 128], qb16[:, s_, 0:128], identb)
                S.copy(QnT[:, st * 512:(st + 1) * 512], pst[:, 0:512], eng="act")
                pst2 = S.psum(6, BF16)
                for s_ in range(4):
                    S.tr(pst2[0:64, s_ * 128:(s_ + 1) * 128], qb16[:, s_, 128:192], identb)
                S.copy(QpT[0:64, st * 512:(st + 1) * 512], pst2[0:64, 0:512])
            SB = (0, 1, 2, 7)
            pend = []

            def epilogue(T_, psO_, psL_):
                qs_ = slice(T_ * 512, (T_ + 1) * 512)
                S.mm(psL_, onesf, dsum, start=False, stop=True)
                S.recip(rec, psL_)
                S.tt(attnT[:, h, qs_], psO_, rec, ALU.mult)

            for T in range(P3T if (P3M & 4) else 0):
                nkb = 8 * T + 8
                psO = S.psum(3 if T % 2 == 0 else 5)
                psLs = S.psum(4 if T % 2 == 0 else 6)

                def c0_of(kb):
                    r = kb - 8 * T
                    return 0 if r < 2 else (r // 2) * 128

                def issue_S(kb):
                    c0 = c0_of(kb)
                    ps = S.psum(SB[kb % 4])
                    S.mm(ps[:, c0:512], KhT[:, kb * 128:(kb + 1) * 128], QnT[:, T * 512 + c0:(T + 1) * 512],
                         start=True, stop=False)
                    S.mm(ps[:, c0:512], RT[:, kb * 128:(kb + 1) * 128], QpT[:, T * 512 + c0:(T + 1) * 512],
                         start=False, stop=True)

                issue_S(0)
                issue_S(1)
                first_pe = True
                for kb in range(nkb):
                    if kb == 5 and pend:
                        epilogue(*pend.pop())
                    bg_convert()
                    if kb + 2 < nkb:
                        issue_S(kb + 2)
                    c0 = c0_of(kb)
                    pt = PT[kb % 4]
                    S.act(pt[:, c0:512], S.psum(SB[kb % 4])[:, c0:512], AF.Exp, scale=sk[:, kb:kb + 1])
                    if kb >= 8 * T:
                        S.tt(pt[:, c0:512], pt[:, c0:512], maskb[:, kb - 8 * T, c0:512], ALU.mult)
                    S.mm(psO[:, c0:512], Vh[:, kb, :], pt[:, c0:512], start=(kb == 0), stop=(kb == nkb - 1))
                    if kb == 0 and T > 0:
                        S.copy(den, pt)
                    elif kb >= 8 * T or kb % 4 == 3:
                        S.mm(psLs[:, c0:512], onesb, pt[:, c0:512], start=first_pe, stop=False)
                        first_pe = False
                    else:
                        S.tt(den, den, pt, ALU.add)
                if T > 0:
                    S.copy(dsum, den)
                else:
                    S.memset(dsum, 0.0)
                pend.append((T, psO, psLs))
            if pend:
                epilogue(*pend.pop())
        if debug:
            S.dma("sp", dout("d_attnT", [128, 4, 4096], BF16), attnT)
        S.release(markA)
        S.free_top(2 * 4096 * 2)
        if stop_after <= 3:
            S.emit()
            return nc, dbg

        NBLK = 96
        X1s = V(X1_d, "X1")
        H2s = V(H2_d, "H2")
        Xgs = V(Xg_d, "Xg")
        Ys = V(Y_d, "Y")
        S.stt(a2, modT[:, 32:40], 1.0, sm[:, C_G2:C_G2 + 8], ALU.add, ALU.mult)
        g1bc = S.alloc("g1bc", [1024], F32)
        g2bc = S.alloc("g2bc", [1024], F32)
        a2bc = S.alloc("a2bc", [1024], F32)
        sh2bc = S.alloc("sh2bc", [1024], F32)
        mg = S.mark()
        Gt = [S.alloc(f"Gt{i}", [128], F32) for i in range(2)]
        n = 0
        for (srcv, dst, banks) in ((modT[:, 16:24], g1bc, (1, 2)), (modT[:, 40:48], g2bc, (3, 4)),
                                   (a2, a2bc, (5, 6)), (modT[:, 24:32], sh2bc, (7, 0))):
            for j in range(8):
                gt = Gt[n % 2]
                n += 1
                S.copy(gt, srcv[:, j:j + 1].bc([128, 128]))
                ps = S.psum(banks[j // 4])
                S.mm(ps[:, (j % 4) * 128:(j % 4 + 1) * 128], gt, identf)
            for bb in range(2):
                S.copy(dst[:, bb * 512:(bb + 1) * 512], S.psum(banks[bb]), eng="act")
        S.release(mg)
        ohA = S.alloc("ohA", [32, 32], F32)
        ohB = S.alloc("ohB", [32, 32], F32)
        rank = S.alloc("rank", [32, 32], F32)
        w12 = S.alloc("w12", [32, 2], F32)
        Msum = S.alloc("Msum", [32], BF16)
        ltrib = S.alloc("ltrib", [128], BF16)
        wrb = S.alloc("wrb", [8, 36], BF16)
        S.dma("pool", wrb, wr_d.rearrange("(k p) n -> p k n", p=128))
        S.dma("pool", ltrib, ltri_d)
        S.memset(Msum, 0.0)
        ma = S.mark()
        woutb = S.alloc("woutb", [8, 1024], BF16)
        S.dma("pool", woutb, wout_d.rearrange("(k p) n -> p k n", p=128))
        mixedT = [S.alloc(f"mixedT{i}", [8, 512], BF16) for i in range(2)]
        sq = S.alloc("sq", [4, 512], BF16)
        sq2 = sq
        rsa = S.alloc("rsa", [512], F32)
        rsl = S.alloc("rsl", [512], F32)
        xt = [S.alloc(f"xt{i}", [1024], F32) for i in range(3)]
        x1t = [S.alloc(f"x1t{i}", [1024], F32) for i in range(4)]
        h2tok = [S.alloc(f"h2tok{i}", [1024], BF16) for i in range(2)]
        h2Tt = [S.alloc(f"h2Tt{i}", [8, 128], BF16) for i in range(2)]
        tmp = [S.alloc(f"tmp{i}", [512], F32) for i in range(4)]
        tmpf = [S.alloc(f"tmpf{i}", [1024], F32) for i in range(2)]
        junk = S.alloc("junk", [1024], BF16)
        st2 = [S.alloc(f"st2_{i}", [2], F32) for i in range(4)]
        diag = [S.alloc(f"diag{i}", [128], F32) for i in range(3)]
        lg = [S.alloc(f"lg{i}", [36], F32) for i in range(4)]
        NR8 = 8
        rsmA = S.alloc("rsmA", [NR8, 8], F32)
        gmaskA = S.alloc("gmaskA", [NR8, 4], F32)
        gexA = S.alloc("gexA", [NR8, 4], F32)
        penA = S.alloc("penA", [NR8, 4], F32)
        elmA = S.alloc("elmA", [NR8, 32], F32)
        m8A = S.alloc("m8A", [NR8, 8], F32)
        MiA = S.alloc("MiA", [NR8, 32], BF16)
        NTA = 32

        def SA0(hb):
            c0 = hb * 512
            cols = slice(c0, c0 + 512)
            mx = mixedT[hb % 2]
            S.tt(sq, attnT[:, :, cols], attnT[:, :, cols], ALU.mult, eng="pool")
            psN = S.psum(7)
            for h in range(4):
                S.mm(psN, onesb, sq[:, h, :], start=(h == 0), stop=(h == 3))
            S.act(rsa, psN, AF.Ln, scale=1.0 / 512, bias=EPS)
            S.act(rsa, rsa, AF.Exp, scale=-0.5)
            for h in range(4):
                S.stt(mx[:, h, :].k(h), attnT[:, h, cols], sm[:, C_GA + h:C_GA + h + 1], rsa, ALU.mult, ALU.mult)
            S.tt(sq2, hown[:, :, cols], hown[:, :, cols], ALU.mult, eng="pool")
            psN2 = S.psum(7)
            for h in range(4):
                S.mm(psN2, onesb, sq2[:, h, :], start=(h == 0), stop=(h == 3))
            S.act(rsl, psN2, AF.Ln, scale=1.0 / 512, bias=EPS)
            S.act(rsl, rsl, AF.Exp, scale=-0.5)
            for c in range(4):
                S.stt(mx[:, 4 + c, :].k(4 + c), hown[:, c, cols], sm[:, C_GL + c:C_GL + c + 1], rsl, ALU.mult, ALU.mult)

        def tv(arr, i, lo=None, hi=None):
            r = i % NR8
            v = arr[:, r, :] if lo is None else arr[:, r, lo:hi]
            return v.k(r)

        def A1(i):
            hb, s_ = i // 4, i % 4
            mx = mixedT[hb % 2]
            xb = xt[i % 3]
            S.dma("sp", xb, xo_d[i * 128:(i + 1) * 128, :])
            for n_ in range(2):
                ps = S.psum((0 if i % 2 else 2) + n_)
                for k in range(8):
                    S.mm(ps, mx[:, k, s_ * 128:(s_ + 1) * 128].k(k), woutb[:, k, n_ * 512:(n_ + 1) * 512],
                         start=(k == 0), stop=(k == 7))

        def A2(i):
            for n_ in range(2):
                ps = S.psum((0 if i % 2 else 2) + n_)
                S.tt(tmp[(i % 2) * 2 + n_], ps, g1bc[:, n_ * 512:(n_ + 1) * 512], ALU.mult)

        def A3(i):
            xb = xt[i % 3]
            x1 = x1t[i % 4]
            for n_ in range(2):
                S.tt(x1[:, n_ * 512:(n_ + 1) * 512].k(n_), tmp[(i % 2) * 2 + n_], xb[:, n_ * 512:(n_ + 1) * 512],
                     ALU.add, eng="pool")
            S.dma("sp", X1s[i * 128:(i + 1) * 128, :].k(i), x1.ks([0, 1]))

        def A4(i):
            x1f = x1t[i % 4].ks([0, 1])
            st = st2[i % 4]
            S.act(junk, x1f, AF.Square, accum_out=st[:, 0:1])
            S.act(st[:, 1:2], st[:, 0:1], AF.Ln, scale=1.0 / 1024, bias=EPS)
            S.act(st[:, 1:2], st[:, 1:2], AF.Exp, scale=-0.5)

        def A5(i):
            x1f = x1t[i % 4].ks([0, 1])
            st = st2[i % 4]
            S.stt(tmpf[i % 2], x1f, st[:, 1:2], a2bc, ALU.mult, ALU.mult)
            S.ts(diag[i % 3], identf, st[:, 1:2], None, ALU.mult)

        def A6(i):
            x1f = x1t[i % 4].ks([0, 1])
            S.tt(h2tok[i % 2], tmpf[i % 2], sh2bc, ALU.add, eng="pool")
            S.dma("sp", H2s[i * 128:(i + 1) * 128, :].k(i), h2tok[i % 2])
            for hf in range(2):
                ps = S.psum(4 + hf)
                for kk in range(4):
                    k = hf * 4 + kk
                    S.mm(ps[:, kk * 128:(kk + 1) * 128], x1f[:, k * 128:(k + 1) * 128], diag[i % 3])

        def A7(i):
            hT_ = h2Tt[i % 2]
            for hf in range(2):
                ps = S.psum(4 + hf)
                for kk in range(4):
                    k = hf * 4 + kk
                    dst = hT_[:, k, :].k(k)
                    src = ps[:, kk * 128:(kk + 1) * 128]
                    if hf == 0:
                        S.act(dst, src, AF.Identity, scale=a2[:, k:k + 1], bias=sh2[:, k:k + 1])
                    else:
                        S.ts(dst, src, a2[:, k:k + 1], sh2[:, k:k + 1], ALU.mult, ALU.add)

        def A8(i):
            hT_ = h2Tt[i % 2]
            psR = S.psum(6)
            for k in range(8):
                S.mm(psR[:, 0:36], hT_[:, k, :].k(k), wrb[:, k, :], start=(k == 0), stop=(k == 7))

        def A9(i):
            lgi = lg[i % 4]
            S.tt(lgi, S.psum(6)[:, 0:36], rows[:, R_BR:R_BR + 36], ALU.add)
            S.v("dve", "tensor_reduce", [tv(rsmA, i)], [lgi], tv(rsmA, i, 0, 1), lgi[:, 0:4], AX.X, ALU.max)
            S.ts(tv(gmaskA, i), lgi[:, 0:4], tv(rsmA, i, 0, 1), None, ALU.is_equal)
            S.ts(tv(rsmA, i, 1, 2), tv(rsmA, i, 0, 1), -1.0, None, ALU.mult)

        def A10(i):
            lgi = lg[i % 4]
            S.act(tv(gexA, i), lgi[:, 0:4], AF.Exp, bias=tv(rsmA, i, 1, 2), accum_out=tv(rsmA, i, 2, 3))

        def A11(i):
            lgi = lg[i % 4]
            r = i % NR8
            S.recip(tv(rsmA, i, 3, 4), tv(rsmA, i, 2, 3))
            S.ts(tv(penA, i), tv(gmaskA, i), 1.0, 1e30, ALU.subtract, ALU.mult)
            elm3 = V(elmA[:, r, :].ap.rearrange("p (g e) -> p g e", g=4), ("elmA", r))
            lg3 = V(lgi[:, 4:36].ap.rearrange("p (g e) -> p g e", g=4), lgi.key)
            penb = V(penA[:, r, :].ap.unsqueeze(2).to_broadcast([128, 4, 8]), ("penA", r))
            S.tt(elm3, lg3, penb, ALU.add)
            S.v("dve", "max", [tv(m8A, i)], [tv(elmA, i)], tv(m8A, i), tv(elmA, i))
            S.tt(tv(rsmA, i, 4, 5), tv(m8A, i, 0, 1), tv(m8A, i, 1, 2), ALU.subtract)

        def A12(i):
            S.act(tv(rsmA, i, 5, 6), tv(rsmA, i, 4, 5), AF.Exp, scale=-1.0)

        def A13(i):
            w1 = w12[:, i, 0:1]
            w2 = w12[:, i, 1:2]
            ee = tv(rsmA, i, 5, 6)
            gp = tv(rsmA, i, 3, 4)
            S.ts(ee, ee, 1.0, None, ALU.add)
            S.recip(ee, ee)
            S.tt(w1, ee, gp, ALU.mult)
            S.tt(w2, gp, w1, ALU.subtract)
            S.ts(ohA[:, i, :], tv(elmA, i), tv(m8A, i, 0, 1), None, ALU.is_equal)
            S.ts(ohB[:, i, :], tv(elmA, i), tv(m8A, i, 1, 2), None, ALU.is_equal)
            S.tt(tv(MiA, i), ohA[:, i, :], ohB[:, i, :], ALU.add)

        def A14(i):
            psK = S.psum(7)
            S.mm(psK[:, 0:32], ltrib, tv(MiA, i), start=True, stop=False)
            S.mm(psK[:, 0:32], onesb, Msum, start=False, stop=True)
            S.copy(rank[:, i, :], psK[:, 0:32], eng="act")
            S.tt(Msum, Msum, tv(MiA, i), ALU.add)

        stages = [A1, A2, A3, A4, A5, A6, A7, A8, A9, A10, A11, A12, A13, A14]
        SA0(0)
        for t in range(NTA + len(stages) - 1):
            for si in range(len(stages) - 1, -1, -1):
                i = t - si
                if 0 <= i < NTA:
                    if si == 0 and t % 4 == 1 and (t + 3) // 4 < NTA // 4:
                        SA0((t + 3) // 4)
                    stages[si](i)
        S.release(ma)
        S.arena_bytes += 4 * 4096 * 2
        mr = S.mark()
        cnt = S.alloc("cnt", [32], F32)
        yv = S.alloc("yv", [32], F32)
        zi = S.alloc("zi", [32], I32)
        zf = S.alloc("zf", [32], F32)
        cum = S.alloc("cum", [32], F32)
        pstart = S.alloc("pstart", [32], F32)
        ones32 = S.alloc("ones32", [32], F32)
        be = S.alloc("be", [NBLK], F32)
        chg = S.alloc("chg", [NBLK], F32)
        t1 = S.alloc("t1", [NBLK], F32)
        idxw = S.alloc("idxw", [NBLK], I32)
        dstf = S.alloc("dstf", [2, 32], F32)
        dsti = S.alloc("dsti", [2, 32], I32)
        mr2 = S.mark()
        cmp3 = S.alloc("cmp3", [NBLK, 32], F32)
        big = S.alloc("big", [32, 32], F32)
        psK = S.psum(7)
        S.mm(psK[:, 0:32], onesb, Msum)
        S.copy(cnt, psK[:, 0:32])
        S.ts(yv, cnt, 1.0 / 128, 127.0 / 128, ALU.mult, ALU.add)
        S.copy(zi, yv)
        S.copy(zf, zi)
        S.tt(yv, zf, yv, ALU.is_gt)
        S.tt(zf, zf, yv, ALU.subtract)
        S.memset(ones32, 1.0)
        S.v("dve", "tensor_tensor_scan", [cum], [ones32, zf], cum, ones32, zf, 0.0, ALU.mult, ALU.add)
        S.tt(pstart, cum, zf, ALU.subtract)
        S.ts(pstart, pstart, 128.0, None, ALU.mult)
        cumb = V(cum.ap.unsqueeze(1).to_broadcast([128, NBLK, 32]), cum.key)
        jb = V(rows[:, R_J:R_J + NBLK].ap.unsqueeze(2).to_broadcast([128, NBLK, 32]), rows.key)
        S.tt(cmp3, cumb, jb, ALU.is_le)
        S.v("dve", "tensor_reduce", [be], [cmp3], be, cmp3, AX.X, ALU.add)
        S.ts(be, be, 31.0, None, ALU.min)
        S.memset(chg, 1.0)
        S.tt(chg[:, 2:NBLK], be[:, 2:NBLK], be[:, 0:NBLK - 2], ALU.not_equal)
        S.ts(t1, be, 128.0, None, ALU.mult)
        S.ts(t1, t1, sm[:, C_PCOL:C_PCOL + 1], None, ALU.add)
        S.ts(chg, chg, -1.0e6, 1.0e6, ALU.mult, ALU.add)
        S.tt(t1, t1, chg, ALU.add)
        S.copy(idxw, t1)
        psb_ = V(pstart.ap.unsqueeze(1).to_broadcast([128, 32, 32]), pstart.key)
        S.tt(big, rank, psb_, ALU.add)
        for q_, oh in enumerate((ohA, ohB)):
            cm = V(cmp3.ap.rearrange("p a b -> p (a b)")[:, 0:1024].rearrange("p (a b) -> p a b", a=32), cmp3.key)
            S.tt(cm, big, oh, ALU.mult)
            S.v("dve", "tensor_reduce", [dstf], [cmp3], dstf[:, q_, :], cm, AX.X, ALU.add)
        S.copy(dsti, dstf)
        S.barrier()
        S.top = mr2
        if debug:
            S.dma("sp", dout("d_cnt", [128, 32]), cnt)
            S.dma("sp", dout("d_be", [128, NBLK]), be)
            S.dma("sp", dout("d_idxw", [128, NBLK], I32), idxw)
            S.dma("sp", dout("d_dst", [128, 2, 32], I32), dsti)
            S.dma("sp", dout("d_w12", [128, 32, 2]), w12)
        hb2 = [S.alloc(f"hb2_{i}", [1024], BF16) for i in range(2)]
        xg_keys = []
        for i in range(NTA):
            t_ = hb2[i % 2]
            S.dma("sp", t_, H2s[i * 128:(i + 1) * 128, :].k(i))
            for q_ in range(2):
                key = ("Xg", i, q_)
                xg_keys.append(key)

                def scat(e, src=t_.ap, ix=dsti.ap, i=i, q_=q_):
                    return e.indirect_dma_start(out=Xg_d[:, :],
                                                out_offset=bass.IndirectOffsetOnAxis(ap=ix[:, q_, i:i + 1], axis=0),
                                                in_=src, in_offset=None)
                S.dma_custom("pool", scat, reads=[t_.key, dsti.key], writes=[key])
        wgu = [S.alloc(f"wgu{i}", [8, 512], BF16) for i in range(2)]
        wdn = [S.alloc(f"wdn{i}", [2, 1024], BF16) for i in range(2)]
        Xb = [S.alloc(f"Xb{i}", [1024], BF16) for i in range(3)]
        XgT = [S.alloc(f"XgT{i}", [8, 128], BF16) for i in range(2)]
        sgl = [S.alloc(f"sgl{i}", [256], F32) for i in range(2)]
        hid = [S.alloc(f"hid{i}", [256], BF16) for i in range(2)]
        hidT = [S.alloc(f"hidT{i}", [2, 128], BF16) for i in range(2)]
        Yb = [S.alloc(f"Yb{i}", [1024], F32) for i in range(2)]
        regs = {}
        y_keys = []

        def _wgather(j, dst_v, src_d):
            def gath(e, o=dst_v.ap.rearrange("p a b -> p (a b)"), src_d=src_d, ix=idxw.ap, j=j):
                if "bc" not in regs:
                    regs["bc"] = e.alloc_register("bcreg")
                    e.reg_mov(regs["bc"], 4095)
                return e.indirect_dma_start(out=o, out_offset=None, in_=src_d[:, :],
                                            in_offset=bass.IndirectOffsetOnAxis(ap=ix[:, j:j + 1], axis=0),
                                            bounds_check=regs["bc"], oob_is_err=False)
            S.dma_custom("pool", gath, reads=[idxw.key] + wb_keys, writes=[dst_v.key])

        def B1wg(j):
            s_ = j % 2
            _wgather(j, wgu[s_][:, 0:4, :].k("lo"), wb_d[0])
            _wgather(j, wgu[s_][:, 4:8, :].k("hi"), wb_d[1])

        def B1wd(j):
            _wgather(j, wdn[j % 2], wb_d[2])

        def B1x(j):
            S.dma_custom("sp", (lambda e, o=Xb[j % 3].ap, j=j: e.dma_start(out=o, in_=Xg_d[j * 128:(j + 1) * 128, :])),
                         reads=xg_keys, writes=[Xb[j % 3].key])

        def B2(j):
            s_ = j % 2
            psX = S.psum(s_, BF16)
            for k in range(8):
                S.tr(psX[:, k * 128:(k + 1) * 128], Xb[j % 3][:, k * 128:(k + 1) * 128], identb)
            S.copy(XgT[s_], V(psX.ap.rearrange("p (k t) -> p k t", k=8), psX.key), eng=("act" if s_ == 0 else "dve"))

        def B3(j):
            s_ = j % 2
            psG = S.psum(2 + s_)
            for k in range(8):
                S.mm(psG, XgT[s_][:, k, :], wgu[s_][:, k, :].k("lo" if k < 4 else "hi"), start=(k == 0), stop=(k == 7))
            S.act(sgl[s_], psG[:, 0:256], AF.Silu)
            S.tt(hid[s_], psG[:, 256:512], sgl[s_], ALU.mult)

        def B4(j):
            s_ = j % 2
            psTt = S.psum(4, BF16)
            for c in range(2):
                S.tr(psTt[:, c * 128:(c + 1) * 128], hid[s_][:, c * 128:(c + 1) * 128], identb)
            S.copy(hidT[s_], V(psTt[:, 0:256].ap.rearrange("p (c t) -> p c t", c=2), psTt.key), eng="act")
            for n_ in range(2):
                psD = S.psum(5 + n_)
                for c in range(2):
                    S.mm(psD, hidT[s_][:, c, :], wdn[s_][:, c, n_ * 512:(n_ + 1) * 512], start=(c == 0), stop=(c == 1))
                if n_ == 0:
                    S.copy(Yb[s_][:, 0:512], psD, eng="act")
                else:
                    S.copy(Yb[s_][:, 512:1024], psD)
            key = ("Y", j)
            y_keys.append(key)
            S.dma_custom("sp", (lambda e, i_=Yb[s_].ap, j=j: e.dma_start(out=Y_d[j * 128:(j + 1) * 128, :], in_=i_)),
                         reads=[Yb[s_].key], writes=[key])

        B1wg(0)
        B1wd(0)
        B1wg(1)
        B1x(0)
        B1x(1)
        B2(0)
        for t in range(NBLK + 1):
            if t + 2 < NBLK:
                B1x(t + 2)
            if t + 1 < NBLK:
                B2(t + 1)
            if t < NBLK:
                B3(t)
                if t + 2 < NBLK:
                    B1wg(t + 2)
            if 0 <= t - 1 < NBLK:
                B4(t - 1)
            if t + 1 < NBLK:
                B1wd(t + 1)
        Y0 = [S.alloc(f"Y0_{i}", [1024], F32) for i in range(2)]
        Y1 = [S.alloc(f"Y1_{i}", [1024], F32) for i in range(2)]
        x1c = [S.alloc(f"x1c{i}", [1024], F32) for i in range(2)]
        for i in range(NTA):
            b_ = i % 2
            for q_, dstt in enumerate((Y0[b_], Y1[b_])):
                def gy(e, o=dstt.ap, ix=dsti.ap, i=i, q_=q_):
                    return e.indirect_dma_start(out=o, out_offset=None, in_=Y_d[:, :],
                                                in_offset=bass.IndirectOffsetOnAxis(ap=ix[:, q_, i:i + 1], axis=0))
                S.dma_custom("pool", gy, reads=y_keys + [dsti.key], writes=[dstt.key])
            S.dma("sp", x1c[b_], X1s[i * 128:(i + 1) * 128, :].k(i))
            S.act(Y0[b_], Y0[b_], AF.Identity, scale=w12[:, i, 0:1])
            S.stt(Y0[b_], Y1[b_], w12[:, i, 1:2], Y0[b_], ALU.mult, ALU.add)
            S.tt(Y0[b_], Y0[b_], g2bc, ALU.mult)
            S.tt(x1c[b_], x1c[b_], Y0[b_], ALU.add)
            S.dma("sp", out_d[i * 128:(i + 1) * 128, :], x1c[b_])
        S.emit()
    return nc, dbg

def prepare_inputs(inp):
    L = 0
    f32 = np.float32
    g = lambda n: np.asarray(inp[n][L], dtype=f32)
    x = np.asarray(inp["x"], dtype=f32)
    c = np.asarray(inp["c"], dtype=f32)
    pos = np.asarray(inp["positions"]).astype(np.int32)
    colT = lambda v, k: np.ascontiguousarray(v.reshape(k, 128).T)
    inv = (1.0 / (10000.0 ** (np.arange(0, 64, 2, dtype=f32) / 64.0))).astype(f32)
    shared = {
        "ident": np.eye(128, dtype=f32),
        "w_ada": g("w_ada"), "w_in": g("w_in"), "w_q_b": g("w_q_b"), "w_kv_b": g("w_kv_b"),
        "lru_wa": g("lru_wa"), "lru_wx": g("lru_wx"), "w_out": g("w_out"),
        "w_router": np.ascontiguousarray(np.concatenate([g("w_router_group"), g("w_router_expert")], axis=1)),
        "ltri": np.triu(np.ones((128, 128), f32), k=1),
    }
    wg = g("w_gate").reshape(32, 8, 128, 256)
    wu = g("w_up").reshape(32, 8, 128, 256)
    wgu = np.concatenate([wg, wu], axis=-1).transpose(0, 2, 1, 3)
    shared["wgu_lo"] = np.ascontiguousarray(wgu[:, :, 0:4, :]).reshape(4096, 2048)
    shared["wgu_hi"] = np.ascontiguousarray(wgu[:, :, 4:8, :]).reshape(4096, 2048)
    shared["wd_l"] = np.ascontiguousarray(g("w_down").reshape(32, 2, 128, 1024).transpose(0, 2, 1, 3)).reshape(4096, 2048)
    rows = np.zeros((NR,), f32)
    rows[R_GQA:R_GQA + 256] = g("q_a_norm")
    rows[R_GKVA:R_GKVA + 128] = g("kv_a_norm")
    rows[R_GQ:R_GQ + 192] = g("q_norm")
    rows[R_GKPE:R_GKPE + 64] = g("k_norm")[128:]
    rows[R_INV:R_INV + 32] = inv
    rows[R_BR:R_BR + 4] = g("b_router_group")
    rows[R_BR + 4:R_BR + 36] = g("b_router_expert")
    rows[R_J:R_J + 96] = np.arange(96, dtype=f32)
    shared["rows"] = np.ascontiguousarray(np.broadcast_to(rows[None, :], (128, NR)))
    maps = []
    for core in range(8):
        b, p = core // 2, core % 2
        sm = np.zeros((128, NS), f32)
        sm[:, C_CT:C_CT + 8] = colT(c[b], 8)
        sm[:, C_BADA:C_BADA + 48] = colT(g("b_ada"), 48)
        sm[:, C_G1:C_G1 + 8] = colT(g("norm1_g"), 8)
        sm[:, C_G2:C_G2 + 8] = colT(g("norm2_g"), 8)
        sm[:, C_KNG] = g("k_norm")[:128]
        cw = g("conv_w")
        for ch in range(4):
            for j in range(4):
                sm[:, C_CW + ch * 4 + j] = cw[j, ch * 128:(ch + 1) * 128]
        sm[:, C_CB:C_CB + 4] = colT(g("conv_b"), 4)
        sm[:, C_BA:C_BA + 4] = colT(g("lru_ba"), 4)
        sm[:, C_BX:C_BX + 4] = colT(g("lru_bx"), 4)
        sm[:, C_LAM:C_LAM + 4] = colT(g("lru_lambda"), 4)
        sm[:, C_GA:C_GA + 4] = colT(g("attn_out_norm"), 4)
        sm[:, C_GL:C_GL + 4] = colT(g("lru_out_norm"), 4)
        sm[:, C_PE] = 1.0 - p
        sm[:, C_PO] = float(p)
        sm[:, C_PCOL] = np.arange(128, dtype=f32)
        xb = x[b]
        own = xb.reshape(32, 2, 128, 1024)[:, p].reshape(4096, 1024)
        pb = pos[b]
        pown = pb.reshape(32, 2, 128)[:, p].reshape(4096)
        mask = np.zeros((128, 8, 512), f32)
        kk = np.arange(128)[:, None]
        qq = np.arange(128)[None, :]
        for r in range(8):
            for j in range(4):
                qb = 2 * j + p
                if r < qb:
                    m = np.ones((128, 128), f32)
                elif r == qb:
                    m = (kk <= qq).astype(f32)
                else:
                    m = np.zeros((128, 128), f32)
                mask[:, r, j * 128:(j + 1) * 128] = m
        d = dict(shared)
        d.update({
            "xf": np.ascontiguousarray(xb), "xo": np.ascontiguousarray(own),
            "posf_pm": np.ascontiguousarray(pb.reshape(64, 128).T),
            "poso_pm": np.ascontiguousarray(pown.reshape(32, 128).T),
            "posf_row": np.ascontiguousarray(pb.reshape(1, 8192)),
            "smalls": sm, "maskf": mask,
        })
        maps.append(d)
    return maps


def assemble(results):
    out = np.zeros((4, 8192, 1024), np.float32)
    ov = out.reshape(4, 32, 2, 128, 1024)
    for core in range(8):
        b, p = core // 2, core % 2
        ov[b, :, p] = np.asarray(results[core]["out"]).reshape(32, 128, 1024)
    return out


_NC_CACHE = {}


def kernel(**inputs):
    if "nc" not in _NC_CACHE:
        _NC_CACHE["nc"] = build_nc()[0]
    nc = _NC_CACHE["nc"]
    maps = prepare_inputs(inputs)
    res = run_bass_kernel_spmd(nc, maps, core_ids=list(range(8)))
    return assemble(res.results)
```

```python
import numpy as np
import concourse.bass as bass
import concourse.mybir as mybir
from contextlib import ExitStack

F32 = mybir.dt.float32
BF16 = mybir.dt.bfloat16
I32 = mybir.dt.int32
U32 = mybir.dt.uint32
U8 = mybir.dt.uint8
AF = mybir.ActivationFunctionType
ALU = mybir.AluOpType
AX = mybir.AxisListType

DT_SIZE = {F32: 4, BF16: 2, I32: 4, U32: 4, U8: 1}


class V:
    __slots__ = ("ap", "key")

    def __init__(self, ap, key):
        self.ap = ap
        self.key = key

    def __getitem__(self, idx):
        return V(self.ap[idx], self.key)

    def k(self, tag):
        base = self.key[0] if isinstance(self.key, tuple) else self.key
        return V(self.ap, (base, tag))

    def ks(self, tags):
        base = self.key[0] if isinstance(self.key, tuple) else self.key
        return V(self.ap, [(base, t) for t in tags])

    def bc(self, shape):
        return V(self.ap.to_broadcast(list(shape)), self.key)

    def re(self, s, **kw):
        return V(self.ap.rearrange(s, **kw), self.key)

    def bitcast(self, dt):
        return V(self.ap.bitcast(dt), self.key)

    @property
    def shape(self):
        return self.ap.shape


def _unwrap(x):
    return x.ap if isinstance(x, V) else x


class DmaGroup:
    def __init__(self, sem):
        self.sem = sem
        self.n = 0


class Sched:
    ENGS = ["pe", "act", "dve", "pool", "sp"]

    def __init__(self, nc, es, arena_bytes=204800, n_dma_sems=33):
        self.nc = nc
        self.es = es
        self.sem = {e: es.enter_context(nc.semaphore(f"sem_{e}")) for e in self.ENGS}
        self.cnt = {e: 0 for e in self.ENGS}
        self.ops = {e: [] for e in self.ENGS}
        self.lastw = {}
        self.readers = {}
        self.obs = {e: {} for e in self.ENGS}
        self.dma_sems = [es.enter_context(nc.semaphore(f"sem_dma{i}")) for i in range(n_dma_sems)]
        self.dma_sem_cnt = [0] * n_dma_sems
        self.dma_next = 0
        self.dma_next_pool = 0
        self.dma_next_sp = 0
        self.rr_n = n_dma_sems
        self.semobj = {}
        for e in self.ENGS:
            self.semobj[("eng", e)] = self.sem[e]
        for i, s in enumerate(self.dma_sems):
            self.semobj[("dma", i)] = s
        self.groups = []
        self.all_tokens = []
        self.arena = es.enter_context(nc.sbuf_tensor("arena", [128, arena_bytes], U8))
        self.arena_bytes = arena_bytes
        self.top = 0
        self.ps = [es.enter_context(nc.psum_tensor(f"psb{i}", [128, 512], F32)) for i in range(8)]

    def alloc(self, name, free_shape, dtype, parts=128):
        n = int(np.prod(free_shape)) * DT_SIZE[dtype]
        n_al = (n + 63) // 64 * 64
        assert self.top + n_al <= self.arena_bytes, f"arena overflow {name}: {self.top}+{n_al}"
        ap = self.arena[0:parts, self.top:self.top + n].bitcast(dtype)
        if len(free_shape) == 2:
            ap = ap.rearrange("p (a b) -> p a b", a=free_shape[0])
        elif len(free_shape) == 3:
            ap = ap.rearrange("p (a b c) -> p a b c", a=free_shape[0], b=free_shape[1])
        self.top += n_al
        return V(ap, name)

    def alloc_top(self, name, free_shape, dtype):
        n = int(np.prod(free_shape)) * DT_SIZE[dtype]
        n_al = (n + 63) // 64 * 64
        self.arena_bytes -= n_al
        assert self.top <= self.arena_bytes, f"arena overflow (top) {name}"
        ap = self.arena[:, self.arena_bytes:self.arena_bytes + n].bitcast(dtype)
        if len(free_shape) == 2:
            ap = ap.rearrange("p (a b) -> p a b", a=free_shape[0])
        return V(ap, name)

    def free_top(self, nbytes):
        self.barrier()
        self.arena_bytes += (nbytes + 63) // 64 * 64

    def mark(self):
        return self.top

    def release(self, m):
        self.barrier()
        self.top = m

    def psum(self, bank, dtype=F32):
        ap = self.ps[bank][:, :]
        if dtype != F32:
            ap = ap.bitcast(dtype)
        return V(ap, f"ps{bank}")

    def _deps(self, eng, reads, writes):
        waits = {}

        def need(tok, raw=False):
            if tok is None:
                return
            sk, val = tok
            if sk == ("eng", "pe") and eng == "pe":
                return
            if sk == ("eng", eng) and not raw:
                return
            if self.obs[eng].get(sk, -1) >= val:
                return
            if waits.get(sk, -1) < val:
                waits[sk] = val

        for k in reads:
            need(self.lastw.get(k), raw=True)
        for k in writes:
            need(self.lastw.get(k))
            for t in self.readers.get(k, ()):
                need(t)
        for sk, val in waits.items():
            self.obs[eng][sk] = val
        return list(waits.items())

    def _commit(self, tok, reads, writes):
        for k in writes:
            self.lastw[k] = tok
            self.readers[k] = []
        for k in reads:
            if k in writes:
                continue
            self.readers.setdefault(k, []).append(tok)

    def op(self, eng, fn, reads, writes):
        reads = [k for k in reads if k is not None]
        writes = [k for k in writes if k is not None]
        writes = writes + [k for k in reads if isinstance(k, str) and k.startswith("ps") and k not in writes]
        waits = self._deps(eng, reads, writes)
        self.cnt[eng] += 1
        tok = (("eng", eng), self.cnt[eng])
        self.ops[eng].append((waits, fn, ("eng", eng), 1))
        self._commit(tok, reads, writes)
        return tok

    def dma(self, eng, out, in_, group=None, **kw):
        reads, writes = self._rw([out], [in_])
        waits = self._deps(eng, reads, writes)
        o, i = _unwrap(out), _unwrap(in_)
        if group is not None:
            group.n += 1
            sk = group.sem
            tok = (sk, float("inf"))
        else:
            idx = self._next_dma_sem(eng)
            sk = ("dma", idx)
            prev = self.dma_sem_cnt[idx]
            if prev > 0 and self.obs[eng].get(sk, -1) < prev:
                waits.append((sk, prev))
                self.obs[eng][sk] = prev
            self.dma_sem_cnt[idx] = prev + 16
            tok = (sk, prev + 16)
        qeng = eng

        def fn(e, o=o, i=i, kw=kw):
            return e.dma_start(out=o, in_=i, **kw)

        self.cnt[eng] += 0
        self.ops[qeng].append((waits, fn, sk, 16))
        self._commit(tok, reads, writes)
        self.all_tokens.append(tok)
        return tok

    def dma_custom(self, eng, fn, reads, writes):
        waits = self._deps(eng, list(reads), list(writes))
        idx = self._next_dma_sem(eng)
        sk = ("dma", idx)
        prev = self.dma_sem_cnt[idx]
        if prev > 0 and self.obs[eng].get(sk, -1) < prev:
            waits.append((sk, prev))
            self.obs[eng][sk] = prev
        self.dma_sem_cnt[idx] = prev + 16
        tok = (sk, prev + 16)
        self.ops[eng].append((waits, fn, sk, 16))
        self._commit(tok, list(reads), list(writes))
        self.all_tokens.append(tok)
        return tok

    def _next_dma_sem(self, eng):
        half = self.rr_n // 2
        if eng == "pool":
            i = self.dma_next_pool
            self.dma_next_pool = (i + 1) % half
            return i
        i = self.dma_next_sp
        self.dma_next_sp = (i + 1) % (self.rr_n - half)
        return half + i

    def new_group(self):
        idx = len(self.dma_sems) - 1 - len(self.groups)
        self.rr_n = idx
        assert self.dma_next < self.rr_n
        g = DmaGroup(("dma", idx))
        self.groups.append(g)
        return g

    def barrier(self):
        toks = [(("eng", e), self.cnt[e]) for e in self.ENGS if self.cnt[e] > 0]
        toks += self.all_tokens
        self.all_tokens = []
        for e in self.ENGS:
            waits = {}
            for sk, val in toks:
                if sk == ("eng", e) and e == "pe":
                    continue
                if self.obs[e].get(sk, -1) >= val:
                    continue
                if waits.get(sk, -1) < val:
                    waits[sk] = val
            for sk, val in waits.items():
                self.obs[e][sk] = val
            if waits:
                self.ops[e].append((list(waits.items()), None, None, 0))
        self.lastw = {}
        self.readers = {}

    def _rw(self, out_list, in_list):
        W, R = [], []
        for lst, dst in ((out_list, W), (in_list, R)):
            for x in lst:
                if isinstance(x, V):
                    if isinstance(x.key, list):
                        dst.extend(x.key)
                    else:
                        dst.append(x.key)
        return R, W

    def mm(self, out, lhsT, rhs, start=True, stop=True, extraR=(), **kw):
        R, W = self._rw([out], [lhsT, rhs])
        if not start:
            R = R + W
        o, l, r = _unwrap(out), _unwrap(lhsT), _unwrap(rhs)
        return self.op("pe", lambda e: e.matmul(o, l, r, start=start, stop=stop, **kw), R + list(extraR), W)

    def tr(self, out, in_, ident):
        R, W = self._rw([out], [in_, ident])
        o, i, d = _unwrap(out), _unwrap(in_), _unwrap(ident)
        return self.op("pe", lambda e: e.transpose(o, i, d), R, W)

    def act(self, out, in_, func, bias=None, scale=None, accum_out=None, eng="act"):
        outs = [out] + ([accum_out] if accum_out is not None else [])
        ins = [in_] + [x for x in (bias, scale) if isinstance(x, V)]
        R, W = self._rw(outs, ins)
        kw = {}
        if bias is not None:
            kw["bias"] = _unwrap(bias)
        if scale is not None:
            kw["scale"] = _unwrap(scale)
        if accum_out is not None:
            kw["accum_out"] = _unwrap(accum_out)
        o, i = _unwrap(out), _unwrap(in_)
        return self.op("act", lambda e: e.activation(o, i, func, **kw), R, W)

    def v(self, eng, method, outs, ins, *args, **kwargs):
        R, W = self._rw(outs, ins)
        a = [_unwrap(x) for x in args]
        k = {n: _unwrap(x) for n, x in kwargs.items()}
        return self.op(eng, lambda e: getattr(e, method)(*a, **k), R, W)

    def tt(self, out, in0, in1, op, eng="dve"):
        return self.v(eng, "tensor_tensor", [out], [in0, in1], out, in0, in1, op)

    def ts(self, out, in0, s1, s2, op0, op1=None, eng="dve", accum_out=None):
        ins = [in0] + [x for x in (s1, s2) if isinstance(x, V)]
        outs = [out] + ([accum_out] if accum_out is not None else [])
        kw = {}
        if op1 is not None:
            kw["op1"] = op1
        if accum_out is not None:
            kw["accum_out"] = accum_out
        return self.v(eng, "tensor_scalar", outs, ins, out, in0, s1, s2, op0, **kw)

    def stt(self, out, in0, scalar, in1, op0, op1, eng="dve"):
        ins = [in0, in1] + ([scalar] if isinstance(scalar, V) else [])
        return self.v(eng, "scalar_tensor_tensor", [out], ins, out, in0, scalar, in1, op0, op1)

    def copy(self, out, in_, eng="dve"):
        if eng == "act":
            return self.act(out, in_, AF.Copy)
        return self.v(eng, "tensor_copy", [out], [in_], out, in_)

    def memset(self, out, val, eng="dve"):
        return self.v(eng, "memset", [out], [], out, val)

    def recip(self, out, in_, eng="dve"):
        return self.v(eng, "reciprocal", [out], [in_], out, in_)

    def emit(self):
        nc = self.nc
        engmap = {"pe": "tensor", "act": "scalar", "dve": "vector", "pool": "gpsimd", "sp": "sync"}
        self.barrier()
        sched = self

        def resolve(sk, val):
            if val == float("inf"):
                for g in sched.groups:
                    if g.sem == sk:
                        return 16 * g.n
                raise RuntimeError("group not found")
            return val

        with nc.Block() as block:
            for ename in self.ENGS:
                ops = self.ops[ename]

                def body(e, ops=ops):
                    for waits, fn, incsem, incval in ops:
                        for sk, val in waits:
                            e.wait_ge(sched.semobj[sk], resolve(sk, val))
                        if fn is not None:
                            ins = fn(e)
                            ins.then_inc(sched.semobj[incsem], incval)

                getattr(block, engmap[ename])(body)
from concourse.bass_utils import run_bass_kernel_spmd
import math
import os

C_CT, C_BADA, C_G1, C_G2, C_KNG, C_CW, C_CB, C_BA, C_BX, C_LAM, C_GA, C_GL, C_PE, C_PO = \
    0, 8, 56, 64, 72, 73, 89, 93, 97, 101, 105, 109, 113, 114
C_PCOL = 115
NS = 116
R_GQA, R_GKVA, R_GQ, R_GKPE, R_INV, R_BR = 0, 256, 384, 576, 640, 672
R_J = 708
NR = 804
EPS = 1e-6
TWO_PI = 2.0 * math.pi
PI_LO = 3.1415925


def build_nc(stop_after=99, debug=False):
    nc = bass.Bass("TRN2", target_bir_lowering=False)

    def din(name, shape, dt=F32):
        return nc.dram_tensor(name, list(shape), dt, kind="ExternalInput").ap()

    xf_d = din("xf", [8192, 1024])
    xo_d = din("xo", [4096, 1024])
    posf_d = din("posf_pm", [128, 64], I32)
    poso_d = din("poso_pm", [128, 32], I32)
    posrow_d = din("posf_row", [1, 8192], I32)
    sm_d = din("smalls", [128, NS])
    rows_d = din("rows", [128, NR])
    ident_d = din("ident", [128, 128])
    mask_d = din("maskf", [128, 8, 512])
    wada_d = din("w_ada", [1024, 6144])
    win_d = din("w_in", [1024, 1472])
    wqb_d = din("w_q_b", [256, 768])
    wkvb_d = din("w_kv_b", [128, 1024])
    wa_d = din("lru_wa", [4, 128, 128])
    wx_d = din("lru_wx", [4, 128, 128])
    wout_d = din("w_out", [1024, 1024])
    wr_d = din("w_router", [1024, 36])
    wgl_d = din("wgu_lo", [4096, 2048])
    wgh_d = din("wgu_hi", [4096, 2048])
    wdl_d = din("wd_l", [4096, 2048])
    ltri_d = din("ltri", [128, 128])
    X1_d = nc.dram_tensor("X1_scr", [4096, 1024], F32, kind="Internal").ap()
    H2_d = nc.dram_tensor("H2_scr", [4096, 1024], BF16, kind="Internal").ap()
    Xg_d = nc.dram_tensor("Xg_scr", [96 * 128, 1024], BF16, kind="Internal").ap()
    Y_d = nc.dram_tensor("Y_scr", [96 * 128, 1024], F32, kind="Internal").ap()
    wb_d = [nc.dram_tensor(f"wb_scr{i}", [4096, 2048], BF16, kind="Internal").ap() for i in range(3)]
    wsrc_d = [wgl_d, wgh_d, wdl_d]
    wb_keys = []
    out_d = nc.dram_tensor("out", [4096, 1024], F32, kind="ExternalOutput").ap()
    dbg = {}

    def dout(name, shape, dt=F32):
        dbg[name] = nc.dram_tensor(name, list(shape), dt, kind="ExternalOutput").ap()
        return dbg[name]

    with ExitStack() as es:
        S = Sched(nc, es, arena_bytes=212480)
        G = S.new_group()
        sm = S.alloc("sm", [NS], F32)
        rows = S.alloc("rows", [NR], F32)
        identf = S.alloc("identf", [128], F32)
        identb = S.alloc("identb", [128], BF16)
        onesf = S.alloc("onesf", [128], F32)
        onesb = S.alloc("onesb", [128], BF16)
        modT = S.alloc("modT", [48], F32)
        a1 = S.alloc("a1", [8], F32)
        a2 = S.alloc("a2", [8], F32)
        sc = S.alloc("sc", [8], F32)
        cA = S.alloc("cA", [4], F32)
        cA2 = S.alloc("cA2", [4], F32)
        hown = S.alloc("hown", [4, 4096], BF16)
        markA = S.mark()
        kvnT = S.alloc("kvnT", [8192], BF16)
        RT = S.alloc("RT", [8192], BF16)
        sspe = S.alloc("sspe", [64], F32)
        coso = S.alloc("coso", [32, 32], F32)
        sino = S.alloc("sino", [32, 32], F32)
        S.dma("sp", sm, sm_d, group=G)
        S.dma("sp", rows, rows_d, group=G)
        S.dma("sp", identf, ident_d, group=G)
        S.copy(identb, identf)
        S.memset(onesf, 1.0)
        S.memset(onesb, 1.0, eng="pool")
        sh1 = modT[:, 0:8]
        sh2 = modT[:, 24:32]

        m0 = S.mark()
        cosf = S.alloc("cosf", [64, 32], F32)
        sinf = S.alloc("sinf", [64, 32], F32)
        m0b = S.mark()
        S.act(sc, sm[:, C_CT:C_CT + 8], AF.Silu)
        psA = S.psum(0)
        wada_v = wada_d.rearrange("(k p) n -> p k n", p=128)
        wt = [S.alloc(f"wada{i}", [8, 1024], F32) for i in range(2)]
        for g in range(2):
            t = wt[g % 2]
            S.dma("sp", t, wada_v[:, :, g * 1024:(g + 1) * 1024])
            for j in range(8):
                col = g * 8 + j
                for k in range(8):
                    S.mm(psA[:, col:col + 1], t[:, k, j * 128:(j + 1) * 128], sc[:, k:k + 1],
                         start=(k == 0), stop=(k == 7))
        S.tt(modT[:, 0:16], psA[:, 0:16], sm[:, C_BADA:C_BADA + 16], ALU.add)
        S.stt(a1, modT[:, 8:16], 1.0, sm[:, C_G1:C_G1 + 8], ALU.add, ALU.mult)
        e4 = S.alloc("e4", [4], F32)
        S.act(e4, sm[:, C_LAM:C_LAM + 4], AF.Exp, scale=-1.0)
        S.act(e4, e4, AF.Ln, bias=1.0)
        S.ts(cA, e4, -8.0, None, ALU.mult)
        S.ts(cA2, e4, -16.0, None, ALU.mult)

        def trig(pos_d, NT, cos_t, sin_t, tag):
            mm_ = S.mark()
            posi = S.alloc(f"posi{tag}", [NT], I32)
            posf = S.alloc(f"posf{tag}", [NT], F32)
            ang = S.alloc(f"ang{tag}", [NT, 32], F32)
            kf = S.alloc(f"kf{tag}", [NT, 32], F32)
            ki = S.alloc(f"ki{tag}", [NT, 32], I32)
            w = S.alloc(f"w{tag}", [NT, 32], F32)
            S.dma("sp", posi, pos_d)
            S.copy(posf, posi)
            pb = V(posf.ap.unsqueeze(2).to_broadcast([128, NT, 32]), posf.key)
            ib = V(rows[:, R_INV:R_INV + 32].ap.unsqueeze(1).to_broadcast([128, NT, 32]), rows.key)
            S.tt(ang, pb, ib, ALU.mult)
            S.ts(kf, ang, 1.0 / TWO_PI, None, ALU.mult)
            S.copy(ki, kf)
            S.copy(kf, ki)
            S.stt(ang, kf, -6.28125, ang, ALU.mult, ALU.add)
            S.stt(ang, kf, -(TWO_PI - 6.28125), ang, ALU.mult, ALU.add)

            def wrap(t):
                S.ts(w, t, math.pi, -TWO_PI, ALU.is_gt, ALU.mult)
                S.tt(t, t, w, ALU.add)
                S.ts(w, t, -math.pi, TWO_PI, ALU.is_lt, ALU.mult)
                S.tt(t, t, w, ALU.add)
                S.ts(t, t, PI_LO, -PI_LO, ALU.min, ALU.max)

            wrap(ang)
            S.act(sin_t, ang, AF.Sin)
            S.ts(ang, ang, math.pi / 2, None, ALU.add)
            wrap(ang)
            S.act(cos_t, ang, AF.Sin)
            S.release(mm_)

        trig(posf_d, 64, cosf, sinf, "f")
        trig(poso_d, 32, coso, sino, "o")
        if debug:
            S.dma("sp", dout("d_cosf", [128, 64, 32]), cosf)
            S.dma("sp", dout("d_sinf", [128, 64, 32]), sinf)
            S.dma("sp", dout("d_cA", [128, 4]), cA)
        S.release(m0b)
        if stop_after <= 0:
            S.emit()
            return nc, dbg

        win = S.alloc("win", [8, 704], BF16)
        S.dma("pool", win, win_d.rearrange("(k p) n -> p k n", p=128)[:, :, 256:960])
        wa = S.alloc("wa", [4, 128], BF16)
        wx = S.alloc("wx", [4, 128], BF16)
        S.dma("pool", wa, wa_d.rearrange("h i j -> i h j"))
        S.dma("pool", wx, wx_d.rearrange("h i j -> i h j"))
        xt = [S.alloc(f"xt{i}", [1024], F32) for i in range(4)]
        junk = S.alloc("junk", [1024], BF16)
        hT = S.alloc("hT", [8, 512], BF16)
        ss4 = S.alloc("ss4", [4], F32)
        rstd4 = S.alloc("rstd4", [4], F32)
        diag = [S.alloc(f"diag{i}", [128], F32) for i in range(2)]
        kvn = S.alloc("kvn", [4, 128], BF16)
        kst = S.alloc("kst", [8], F32)
        kpg = S.alloc("kpg", [4, 64], F32)
        Rtm = S.alloc("Rtm", [4, 64], BF16)
        rt1 = S.alloc("rt1", [4, 32], F32)
        rt2 = S.alloc("rt2", [4, 32], F32)
        xbuf = S.alloc("xbuf", [4, 515], F32)
        xc = S.alloc("xc", [4, 512], F32)
        xcb = S.alloc("xcb", [4, 512], BF16)
        rr = S.alloc("rr", [4, 512], F32)
        ii = S.alloc("ii", [4, 512], F32)
        mm_t = S.alloc("mm_t", [4, 512], F32)
        hh = S.alloc("hh", [4, 512], F32)
        carry = S.alloc("carry", [4], F32)
        posb = S.alloc("posb", [512], I32)
        keep = S.alloc("keep", [512], F32)
        seltmp = S.alloc("seltmp", [2, 128], F32)
        S.memset(xbuf, 0.0)
        S.memset(carry, 0.0)
        S.memset(RT[64:128, :], 0.0, eng="pool")
        pe_c = sm[:, C_PE:C_PE + 1]
        po_c = sm[:, C_PO:C_PO + 1]
        psT = [S.psum(1), S.psum(2)]
        psKV = S.psum(3)
        psL = [S.psum(4), S.psum(5), S.psum(6), S.psum(7)]
        hTs = [hT, S.alloc("hT_b", [8, 512], BF16)]

        def P1(st):
            hT = hTs[st % 2]
            for s in range(4):
                if s > 0:
                    yield
                ti = st * 4 + s
                xb = xt[ti % 4]
                S.dma("sp", xb, xf_d[ti * 128:(ti + 1) * 128, :])
                S.act(junk, xb, AF.Square, accum_out=ss4[:, s:s + 1])
                S.act(rstd4[:, s:s + 1], ss4[:, s:s + 1], AF.Ln, scale=1.0 / 1024, bias=EPS)
                S.act(rstd4[:, s:s + 1], rstd4[:, s:s + 1], AF.Exp, scale=-0.5)
                dg = diag[ti % 2]
                S.ts(dg, identf, rstd4[:, s:s + 1], None, ALU.mult)
                for half in range(2):
                    ps = psT[half]
                    for kk in range(4):
                        k = half * 4 + kk
                        S.mm(ps[:, kk * 128:(kk + 1) * 128], xb[:, k * 128:(k + 1) * 128], dg)
                    for kk in range(4):
                        k = half * 4 + kk
                        dst = hT[:, k, s * 128:(s + 1) * 128].k((k, s))
                        src = ps[:, kk * 128:(kk + 1) * 128]
                        if half == 0:
                            S.act(dst, src, AF.Identity, scale=a1[:, k:k + 1], bias=sh1[:, k:k + 1])
                        else:
                            S.ts(dst, src, a1[:, k:k + 1], sh1[:, k:k + 1], ALU.mult, ALU.add)
            yield

        def P2(st):
            hT = hTs[st % 2]
            for c in range(4):
                for k in range(8):
                    S.mm(psL[c], win[:, k, 192 + c * 128:192 + (c + 1) * 128], hT[:, k, :].ks([(k, q) for q in range(4)]),
                         start=(k == 0), stop=(k == 7))
            yield
            for s in range(4):
                if s > 0:
                    yield
                ti = st * 4 + s
                o = (s % 2) * 192
                for k in range(8):
                    S.mm(psKV[:, o:o + 192], hT[:, k, s * 128:(s + 1) * 128].k((k, s)), win[:, k, 0:192],
                         start=(k == 0), stop=(k == 7))
                S.act(junk[:, 0:128], psKV[:, o:o + 128], AF.Square, accum_out=kst[:, s:s + 1])
                S.act(kst[:, 4 + s:5 + s], kst[:, s:s + 1], AF.Ln, scale=1.0 / 128, bias=EPS)
                S.act(kst[:, 4 + s:5 + s], kst[:, 4 + s:5 + s], AF.Exp, scale=-0.5)
                S.stt(kvn[:, s, :], psKV[:, o:o + 128], kst[:, 4 + s:5 + s], rows[:, R_GKVA:R_GKVA + 128],
                      ALU.mult, ALU.mult)
                S.act(junk[:, 128:192], psKV[:, o + 128:o + 192], AF.Square, accum_out=sspe[:, ti:ti + 1])
                S.tt(kpg[:, s, :], psKV[:, o + 128:o + 192], rows[:, R_GKPE:R_GKPE + 64], ALU.mult)
            yield
            cs = cosf[:, st * 4:(st + 1) * 4, :]
            sn = sinf[:, st * 4:(st + 1) * 4, :]
            x1 = kpg[:, :, 0:32]
            x2 = kpg[:, :, 32:64]
            S.tt(rt1, x1, cs, ALU.mult)
            S.tt(rt2, x2, sn, ALU.mult)
            S.tt(Rtm[:, :, 0:32], rt1, rt2, ALU.subtract)
            S.tt(rt1, x2, cs, ALU.mult)
            S.tt(rt2, x1, sn, ALU.mult)
            S.tt(Rtm[:, :, 32:64], rt1, rt2, ALU.add)
            pst = S.psum(0, BF16)
            for s in range(4):
                S.tr(pst[:, s * 128:(s + 1) * 128], kvn[:, s, :], identb)
            S.copy(kvnT[:, st * 512:(st + 1) * 512], pst[:, 0:512], eng="act")
            pst2 = S.psum(0, BF16)
            for s in range(4):
                S.tr(pst2[0:64, s * 128:(s + 1) * 128], Rtm[:, s, :], identb)
            S.copy(RT[0:64, st * 512:(st + 1) * 512], pst2[0:64, 0:512])
            yield
            S.dma("sp", posb, posrow_d[:, st * 512:(st + 1) * 512].partition_broadcast(128))
            S.ts(keep, posb, 0.0, None, ALU.not_equal)
            for c in range(4):
                xb_c = xbuf[:, c, :].k(c)
                S.copy(xb_c[:, 3:515], psL[c], eng="act")
                cw = lambda j: sm[:, C_CW + c * 4 + j:C_CW + c * 4 + j + 1]
                S.ts(xc[:, c, :].k(c), xb_c[:, 3:515], cw(3), sm[:, C_CB + c:C_CB + c + 1], ALU.mult, ALU.add)
                for j in range(3):
                    S.stt(xc[:, c, :].k(c), xb_c[:, j:j + 512], cw(j), xc[:, c, :].k(c), ALU.mult, ALU.add)
                S.copy(xb_c[:, 0:3], xb_c[:, 512:515])
                S.copy(xcb[:, c, :].k(c), xc[:, c, :].k(c), eng="act")
                yield
            for c in range(4):
                S.mm(psL[c], wa[:, c, :], xcb[:, c, :].k(c))
            for c in range(4):
                S.act(rr[:, c, :].k(c), psL[c], AF.Sigmoid, bias=sm[:, C_BA + c:C_BA + c + 1])
            yield
            for c in range(4):
                S.mm(psL[c], wx[:, c, :], xcb[:, c, :].k(c))
            for c in range(4):
                S.act(ii[:, c, :].k(c), psL[c], AF.Sigmoid, bias=sm[:, C_BX + c:C_BX + c + 1])
            yield
            for c in range(4):
                S.act(mm_t[:, c, :].k(c), rr[:, c, :].k(c), AF.Exp, scale=cA2[:, c:c + 1])
                S.act(rr[:, c, :].k(c), rr[:, c, :].k(c), AF.Exp, scale=cA[:, c:c + 1])
            for c in range(4):
                S.ts(mm_t[:, c, :].k(c), mm_t[:, c, :].k(c), 0.9999999, None, ALU.min)
                S.act(mm_t[:, c, :].k(c), mm_t[:, c, :].k(c), AF.Ln, scale=-1.0, bias=1.0)
                S.act(mm_t[:, c, :].k(c), mm_t[:, c, :].k(c), AF.Exp, scale=0.5)
            yield
            for c in range(4):
                if c > 0:
                    yield
                S.tt(ii[:, c, :].k(c), ii[:, c, :].k(c), xc[:, c, :].k(c), ALU.mult)
                S.tt(rr[:, c, :].k(c), rr[:, c, :].k(c), keep, ALU.mult)
                S.stt(mm_t[:, c, :].k(c), mm_t[:, c, :].k(c), -1.0, keep, ALU.add, ALU.mult)
                S.stt(ii[:, c, :].k(c), mm_t[:, c, :].k(c), 1.0, ii[:, c, :].k(c), ALU.add, ALU.mult)
                S.v("dve", "tensor_tensor_scan", [hh[:, c, :].k(c)], [rr[:, c, :].k(c), ii[:, c, :].k(c), carry[:, c:c + 1]],
                    hh[:, c, :].k(c), rr[:, c, :].k(c), ii[:, c, :].k(c), carry[:, c:c + 1], ALU.mult, ALU.add)
                S.copy(carry[:, c:c + 1], hh[:, c, 511:512].k(c))
                hv = V(hh[:, c, :].ap.rearrange("p (j q t) -> p j q t", j=2, q=2), ("hh", c))
                S.ts(seltmp, hv[:, :, 1, :], po_c, None, ALU.mult)
                dst = V(hown[:, c, st * 256:(st + 1) * 256].ap.rearrange("p (j t) -> p j t", j=2), hown.key)
                S.stt(dst, hv[:, :, 0, :], pe_c, seltmp, ALU.mult, ALU.add)

            yield

        def interleave(ga, gb, ratio):
            da = db = False
            while not (da and db):
                for _ in range(ratio):
                    if not da:
                        try:
                            next(ga)
                        except StopIteration:
                            da = True
                if not db:
                    try:
                        next(gb)
                    except StopIteration:
                        db = True

        for _ in P1(0):
            pass
        for st in range(16):
            if st + 1 < 16:
                interleave(P2(st), P1(st + 1), 1)
            else:
                for _ in P2(st):
                    pass
        if debug:
            S.dma("sp", dout("d_kvnT", [128, 8192], BF16), kvnT)
            S.dma("sp", dout("d_RT", [64, 8192], BF16), RT[0:64, :])
            S.dma("sp", dout("d_hown", [128, 4, 4096], BF16), hown)
            S.dma("sp", dout("d_sspe", [128, 64]), sspe)
        S.release(m0)
        if stop_after <= 1:
            S.emit()
            return nc, dbg

        attnT = S.alloc_top("attnT", [4, 4096], BF16)
        qnT = S.alloc_top("qnT", [2, 4096], BF16)
        m2 = S.mark()
        winq = S.alloc("winq", [8, 256], BF16)
        wing = S.alloc("wing", [8, 512], BF16)
        winv = win_d.rearrange("(k p) n -> p k n", p=128)
        S.dma("pool", winq, winv[:, :, 0:256])
        S.dma("pool", wing, winv[:, :, 960:1472])
        xt = [S.alloc(f"xt{i}", [1024], F32) for i in range(4)]
        junk = S.alloc("junk", [1024], BF16)
        hTs2 = [S.alloc(f"hT2_{i}", [8, 512], BF16) for i in range(2)]
        ss4 = S.alloc("ss4", [4], F32)
        rstd4 = S.alloc("rstd4", [4], F32)
        diag = [S.alloc(f"diag{i}", [128], F32) for i in range(2)]
        qst = S.alloc("qst", [8], F32)
        qnb = S.alloc("qnb", [4, 256], BF16)
        gxs = [S.alloc(f"gx{i}", [512], F32) for i in range(2)]
        gus = [S.alloc(f"gu{i}", [512], F32) for i in range(2)]
        wadab = S.alloc("wadab", [8, 512], F32)
        psT = [S.psum(1), S.psum(2)]
        psQ = S.psum(3)
        psL = [S.psum(4), S.psum(5), S.psum(6), S.psum(7)]
        psA0 = S.psum(0)
        GC = 2.0 * math.sqrt(2.0 / math.pi)

        def P1b(st):
            hT = hTs2[st % 2]
            for s_ in range(4):
                if s_ > 0:
                    yield
                ti = st * 4 + s_
                xb = xt[ti % len(xt)]
                S.dma("sp", xb, xo_d[ti * 128:(ti + 1) * 128, :])
                S.act(junk, xb, AF.Square, accum_out=ss4[:, s_:s_ + 1])
                S.act(rstd4[:, s_:s_ + 1], ss4[:, s_:s_ + 1], AF.Ln, scale=1.0 / 1024, bias=EPS)
                S.act(rstd4[:, s_:s_ + 1], rstd4[:, s_:s_ + 1], AF.Exp, scale=-0.5)
                dg = diag[ti % 2]
                S.ts(dg, identf, rstd4[:, s_:s_ + 1], None, ALU.mult)
                for half in range(2):
                    ps = psT[half]
                    for kk in range(4):
                        k = half * 4 + kk
                        S.mm(ps[:, kk * 128:(kk + 1) * 128], xb[:, k * 128:(k + 1) * 128], dg)
                    for kk in range(4):
                        k = half * 4 + kk
                        dst = hT[:, k, s_ * 128:(s_ + 1) * 128].k((k, s_))
                        src = ps[:, kk * 128:(kk + 1) * 128]
                        if half == 0:
                            S.act(dst, src, AF.Identity, scale=a1[:, k:k + 1], bias=sh1[:, k:k + 1])
                        else:
                            S.ts(dst, src, a1[:, k:k + 1], sh1[:, k:k + 1], ALU.mult, ALU.add)
            yield

        def P2b(st):
            hT = hTs2[st % 2]
            for c in range(4):
                for k in range(8):
                    S.mm(psL[c], wing[:, k, c * 128:(c + 1) * 128], hT[:, k, :].ks([(k, q) for q in range(4)]),
                         start=(k == 0), stop=(k == 7))
            yield
            for s_ in range(4):
                ti = st * 4 + s_
                o = (s_ % 2) * 256
                for k in range(8):
                    S.mm(psQ[:, o:o + 256], hT[:, k, s_ * 128:(s_ + 1) * 128].k((k, s_)), winq[:, k, :],
                         start=(k == 0), stop=(k == 7))
                S.act(junk[:, 0:256], psQ[:, o:o + 256], AF.Square, accum_out=qst[:, s_:s_ + 1])
                S.act(qst[:, 4 + s_:5 + s_], qst[:, s_:s_ + 1], AF.Ln, scale=1.0 / 256, bias=EPS)
                S.act(qst[:, 4 + s_:5 + s_], qst[:, 4 + s_:5 + s_], AF.Exp, scale=-0.5)
                S.stt(qnb[:, s_, :], psQ[:, o:o + 256], qst[:, 4 + s_:5 + s_], rows[:, R_GQA:R_GQA + 256],
                      ALU.mult, ALU.mult)
                yield
            for c in range(2):
                pst = S.psum(0, BF16)
                for s_ in range(4):
                    S.tr(pst[:, s_ * 128:(s_ + 1) * 128], qnb[:, s_, c * 128:(c + 1) * 128], identb)
                S.copy(qnT[:, c, st * 512:(st + 1) * 512], pst[:, 0:512], eng=("act" if c == 0 else "dve"))
            yield
            for c in range(4):
                gx, gu = gxs[c % 2], gus[c % 2]
                S.act(gx, psL[c], AF.Copy)
                S.act(gu, psL[c], AF.Square)
                S.ts(gu, gu, 0.044715, 1.0, ALU.mult, ALU.add)
                S.tt(gu, gu, gx, ALU.mult)
                S.act(gu, gu, AF.Sigmoid, scale=GC)
                S.tt(gu, gu, gx, ALU.mult)
                S.tt(hown[:, c, st * 512:(st + 1) * 512], hown[:, c, st * 512:(st + 1) * 512], gu, ALU.mult)
                yield

        def ADA(hg):
            g, hh_ = 2 + hg // 2, hg % 2
            S.dma("sp", wadab, wada_v[:, :, g * 1024 + hh_ * 512:g * 1024 + (hh_ + 1) * 512])
            yield
            for j4 in range(4):
                col = 300 + g * 8 + hh_ * 4 + j4
                for k in range(8):
                    S.mm(psA0[:, col:col + 1], wadab[:, k, j4 * 128:(j4 + 1) * 128], sc[:, k:k + 1],
                         start=(k == 0), stop=(k == 7))
                yield
            c0_ = g * 8 + hh_ * 4
            S.tt(modT[:, c0_:c0_ + 4], psA0[:, 300 + c0_:300 + c0_ + 4], sm[:, C_BADA + c0_:C_BADA + c0_ + 4], ALU.add)
            yield

        def interleave3(ga, gb, gc):
            gens = [ga, gb, gc]
            done = [g is None for g in gens]
            pattern = [0, 1, 2]
            while not all(done):
                for gi in pattern:
                    if not done[gi]:
                        try:
                            next(gens[gi])
                        except StopIteration:
                            done[gi] = True

        for _ in P1b(0):
            pass
        for st in range(8):
            interleave3(P2b(st), P1b(st + 1) if st + 1 < 8 else None, ADA(st))
        if debug:
            S.dma("sp", dout("d_qnT", [128, 2, 4096], BF16), qnT)
            S.dma("sp", dout("d_lru", [128, 4, 4096], BF16), hown)
        S.release(m2)
        if stop_after <= 2:
            S.emit()
            return nc, dbg

        m3 = S.mark()
        wkvb = S.alloc("wkvb", [1024], BF16)
        wqb = S.alloc("wqb", [2, 768], BF16)
        S.dma("pool", wkvb, wkvb_d)
        S.dma("pool", wqb, wqb_d.rearrange("(c p) n -> p c n", p=128))
        maskb = S.alloc("maskb", [8, 512], BF16)
        for r_ in range(8):
            S.dma("pool", maskb[:, r_, :], mask_d[:, r_, :])
        KhT = S.alloc("KhT", [8192], BF16)
        Vh = S.alloc("Vh", [64, 128], BF16)
        QnT = S.alloc("QnT", [4096], BF16)
        QpT = S.alloc("QpT", [4096], BF16)
        ssk = S.alloc("ssk", [64], F32)
        sk = S.alloc("sk", [64], F32)
        junk = S.alloc("junk", [256], BF16)
        qst = S.alloc("qst", [8], F32)
        qg = S.alloc("qg", [4, 192], F32)
        qb16 = S.alloc("qb16", [4, 192], BF16)
        rt1 = S.alloc("rt1", [4, 32], F32)
        rt2 = S.alloc("rt2", [4, 32], F32)
        S.memset(QpT[64:128, :], 0.0, eng="pool")
        wstage = S.alloc("wstage", [2048], BF16)
        bg = {"it": 0}

        def bg_convert():
            it = bg["it"]
            bg["it"] += 1
            c = it // 12
            if c >= 96:
                return
            a_, e_ = c % 3, c // 3
            if it % 12 == 0:
                S.dma("pool", wstage, wsrc_d[a_][e_ * 128:(e_ + 1) * 128, :])
            elif it % 12 == 6:
                key = ("WB", c)
                wb_keys.append(key)
                S.dma("sp", V(wb_d[a_][e_ * 128:(e_ + 1) * 128, :], key), wstage)

        PT = [S.alloc(f"PT{i}", [512], BF16) for i in range(4)]
        den = S.alloc("den", [512], F32)
        rec = S.alloc("rec", [512], F32)
        dsum = S.alloc("dsum", [512], F32)
        SCL = 192.0 ** -0.5
        P3H = int(os.environ.get("P3H", "4"))
        P3T = int(os.environ.get("P3T", "8"))
        P3M = int(os.environ.get("P3M", "7"))
        for h in range(P3H):
            qgf = V(qg.ap.rearrange("p a b -> p (a b)"), qg.key)
            for i2 in range(32 if (P3M & 1) else 0):
                ps = S.psum(3 + (i2 % 4))
                for u in range(2):
                    i = i2 * 2 + u
                    S.mm(ps[:, u * 256:(u + 1) * 256], kvnT[:, i * 128:(i + 1) * 128], wkvb[:, h * 256:(h + 1) * 256])
                ps3 = V(ps.ap.rearrange("p (u c) -> p u c", u=2), ps.key)
                sqv = V(qgf[:, (i2 % 2) * 256:(i2 % 2) * 256 + 256].ap.rearrange("p (u c) -> p u c", u=2), qg.key)
                S.act(sqv, ps3[:, :, 0:128], AF.Square)
                S.act(Vh[:, i2 * 2:i2 * 2 + 2, :], ps3[:, :, 128:256], AF.Copy)
                S.v("dve", "tensor_reduce", [ssk], [sqv], ssk[:, i2 * 2:i2 * 2 + 2], sqv, AX.X, ALU.add)
            for n in range(16 if (P3M & 1) else 0):
                ps = S.psum(5 + (n % 2))
                S.mm(ps, wkvb[:, h * 256:h * 256 + 128], kvnT[:, n * 512:(n + 1) * 512])
                if n % 2 == 0:
                    S.act(KhT[:, n * 512:(n + 1) * 512], ps, AF.Identity, scale=sm[:, C_KNG:C_KNG + 1])
                else:
                    S.ts(KhT[:, n * 512:(n + 1) * 512], ps, sm[:, C_KNG:C_KNG + 1], None, ALU.mult)
            S.tt(sk, ssk, sspe, ALU.add)
            S.act(sk, sk, AF.Ln, scale=1.0 / 192, bias=EPS)
            S.act(sk, sk, AF.Exp, scale=-0.5)
            S.ts(sk, sk, SCL, None, ALU.mult)
            for st in range(8 if (P3M & 2) else 0):
                for s_ in range(4):
                    ti = st * 4 + s_
                    ps = S.psum(7 if s_ < 2 else 4)
                    o = (s_ % 2) * 192
                    for c in range(2):
                        S.mm(ps[:, o:o + 192], qnT[:, c, ti * 128:(ti + 1) * 128], wqb[:, c, h * 192:(h + 1) * 192],
                             start=(c == 0), stop=(c == 1))
                for s_ in range(4):
                    ps = S.psum(7 if s_ < 2 else 4)
                    o = (s_ % 2) * 192
                    S.act(junk[:, 0:192], ps[:, o:o + 192], AF.Square, accum_out=qst[:, s_:s_ + 1])
                S.act(qst[:, 4:8], qst[:, 0:4], AF.Ln, scale=1.0 / 192, bias=EPS)
                S.act(qst[:, 4:8], qst[:, 4:8], AF.Exp, scale=-0.5)
                for s_ in range(4):
                    ps = S.psum(7 if s_ < 2 else 4)
                    o = (s_ % 2) * 192
                    S.stt(qg[:, s_, :], ps[:, o:o + 192], qst[:, 4 + s_:5 + s_], rows[:, R_GQ:R_GQ + 192],
                          ALU.mult, ALU.mult)
                cs = coso[:, st * 4:(st + 1) * 4, :]
                sn = sino[:, st * 4:(st + 1) * 4, :]
                x1 = qg[:, :, 128:160]
                x2 = qg[:, :, 160:192]
                S.copy(qb16[:, :, 0:128], qg[:, :, 0:128])
                S.tt(rt1, x1, cs, ALU.mult)
                S.tt(rt2, x2, sn, ALU.mult)
                S.tt(qb16[:, :, 128:160], rt1, rt2, ALU.subtract)
                S.tt(rt1, x2, cs, ALU.mult)
                S.tt(rt2, x1, sn, ALU.mult)
                S.tt(qb16[:, :, 160:192], rt1, rt2, ALU.add)
                pst = S.psum(5, BF16)
                for s_ in range(4):
                    S.tr(pst[:, s_ * 128:(s_ + 1) * 128], qb16[:, s_, 0:128], identb)
                S.copy(QnT[:, st * 512:(st + 1) * 512], pst[:, 0:512], eng="act")
                pst2 = S.psum(6, BF16)
                for s_ in range(4):
                    S.tr(pst2[0:64, s_ * 128:(s_ + 1) * 128], qb16[:, s_, 128:192], identb)
                S.copy(QpT[0:64, st * 512:(st + 1) * 512], pst2[0:64, 0:512])
            SB = (0, 1, 2, 7)
            pend = []

            def epilogue(T_, psO_, psL_):
                qs_ = slice(T_ * 512, (T_ + 1) * 512)
                S.mm(psL_, onesf, dsum, start=False, stop=True)
                S.recip(rec, psL_)
                S.tt(attnT[:, h, qs_], psO_, rec, ALU.mult)

            for T in range(P3T if (P3M & 4) else 0):
                nkb = 8 * T + 8
                psO = S.psum(3 if T % 2 == 0 else 5)
                psLs = S.psum(4 if T % 2 == 0 else 6)

                def c0_of(kb):
                    r = kb - 8 * T
                    return 0 if r < 2 else (r // 2) * 128

                def issue_S(kb):
                    c0 = c0_of(kb)
                    ps = S.psum(SB[kb % 4])
                    S.mm(ps[:, c0:512], KhT[:, kb * 128:(kb + 1) * 128], QnT[:, T * 512 + c0:(T + 1) * 512],
                         start=True, stop=False)
                    S.mm(ps[:, c0:512], RT[:, kb * 128:(kb + 1) * 128], QpT[:, T * 512 + c0:(T + 1) * 512],
                         start=False, stop=True)

                issue_S(0)
                issue_S(1)
                first_pe = True
                for kb in range(nkb):
                    if kb == 5 and pend:
                        epilogue(*pend.pop())
                    bg_convert()
                    if kb + 2 < nkb:
                        issue_S(kb + 2)
                    c0 = c0_of(kb)
                    pt = PT[kb % 4]
                    S.act(pt[:, c0:512], S.psum(SB[kb % 4])[:, c0:512], AF.Exp, scale=sk[:, kb:kb + 1])
                    if kb >= 8 * T:
                        S.tt(pt[:, c0:512], pt[:, c0:512], maskb[:, kb - 8 * T, c0:512], ALU.mult)
                    S.mm(psO[:, c0:512], Vh[:, kb, :], pt[:, c0:512], start=(kb == 0), stop=(kb == nkb - 1))
                    if kb == 0 and T > 0:
                        S.copy(den, pt)
                    elif kb >= 8 * T or kb % 4 == 3:
                        S.mm(psLs[:, c0:512], onesb, pt[:, c0:512], start=first_pe, stop=False)
                        first_pe = False
                    else:
                        S.tt(den, den, pt, ALU.add)
                if T > 0:
                    S.copy(dsum, den)
                else:
                    S.memset(dsum, 0.0)
                pend.append((T, psO, psLs))
            if pend:
                epilogue(*pend.pop())
        if debug:
            S.dma("sp", dout("d_attnT", [128, 4, 4096], BF16), attnT)
        S.release(markA)
        S.free_top(2 * 4096 * 2)
        if stop_after <= 3:
            S.emit()
            return nc, dbg

        NBLK = 96
        X1s = V(X1_d, "X1")
        H2s = V(H2_d, "H2")
        Xgs = V(Xg_d, "Xg")
        Ys = V(Y_d, "Y")
        S.stt(a2, modT[:, 32:40], 1.0, sm[:, C_G2:C_G2 + 8], ALU.add, ALU.mult)
        g1bc = S.alloc("g1bc", [1024], F32)
        g2bc = S.alloc("g2bc", [1024], F32)
        a2bc = S.alloc("a2bc", [1024], F32)
        sh2bc = S.alloc("sh2bc", [1024], F32)
        mg = S.mark()
        Gt = [S.alloc(f"Gt{i}", [128], F32) for i in range(2)]
        n = 0
        for (srcv, dst, banks) in ((modT[:, 16:24], g1bc, (1, 2)), (modT[:, 40:48], g2bc, (3, 4)),
                                   (a2, a2bc, (5, 6)), (modT[:, 24:32], sh2bc, (7, 0))):
            for j in range(8):
                gt = Gt[n % 2]
                n += 1
                S.copy(gt, srcv[:, j:j + 1].bc([128, 128]))
                ps = S.psum(banks[j // 4])
                S.mm(ps[:, (j % 4) * 128:(j % 4 + 1) * 128], gt, identf)
            for bb in range(2):
                S.copy(dst[:, bb * 512:(bb + 1) * 512], S.psum(banks[bb]), eng="act")
        S.release(mg)
        ohA = S.alloc("ohA", [32, 32], F32)
        ohB = S.alloc("ohB", [32, 32], F32)
        rank = S.alloc("rank", [32, 32], F32)
        w12 = S.alloc("w12", [32, 2], F32)
        Msum = S.alloc("Msum", [32], BF16)
        ltrib = S.alloc("ltrib", [128], BF16)
        wrb = S.alloc("wrb", [8, 36], BF16)
        S.dma("pool", wrb, wr_d.rearrange("(k p) n -> p k n", p=128))
        S.dma("pool", ltrib, ltri_d)
        S.memset(Msum, 0.0)
        ma = S.mark()
        woutb = S.alloc("woutb", [8, 1024], BF16)
        S.dma("pool", woutb, wout_d.rearrange("(k p) n -> p k n", p=128))
        mixedT = [S.alloc(f"mixedT{i}", [8, 512], BF16) for i in range(2)]
        sq = S.alloc("sq", [4, 512], BF16)
        sq2 = sq
        rsa = S.alloc("rsa", [512], F32)
        rsl = S.alloc("rsl", [512], F32)
        xt = [S.alloc(f"xt{i}", [1024], F32) for i in range(3)]
        x1t = [S.alloc(f"x1t{i}", [1024], F32) for i in range(4)]
        h2tok = [S.alloc(f"h2tok{i}", [1024], BF16) for i in range(2)]
        h2Tt = [S.alloc(f"h2Tt{i}", [8, 128], BF16) for i in range(2)]
        tmp = [S.alloc(f"tmp{i}", [512], F32) for i in range(4)]
        tmpf = [S.alloc(f"tmpf{i}", [1024], F32) for i in range(2)]
        junk = S.alloc("junk", [1024], BF16)
        st2 = [S.alloc(f"st2_{i}", [2], F32) for i in range(4)]
        diag = [S.alloc(f"diag{i}", [128], F32) for i in range(3)]
        lg = [S.alloc(f"lg{i}", [36], F32) for i in range(4)]
        NR8 = 8
        rsmA = S.alloc("rsmA", [NR8, 8], F32)
        gmaskA = S.alloc("gmaskA", [NR8, 4], F32)
        gexA = S.alloc("gexA", [NR8, 4], F32)
        penA = S.alloc("penA", [NR8, 4], F32)
        elmA = S.alloc("elmA", [NR8, 32], F32)
        m8A = S.alloc("m8A", [NR8, 8], F32)
        MiA = S.alloc("MiA", [NR8, 32], BF16)
        NTA = 32

        def SA0(hb):
            c0 = hb * 512
            cols = slice(c0, c0 + 512)
            mx = mixedT[hb % 2]
            S.tt(sq, attnT[:, :, cols], attnT[:, :, cols], ALU.mult, eng="pool")
            psN = S.psum(7)
            for h in range(4):
                S.mm(psN, onesb, sq[:, h, :], start=(h == 0), stop=(h == 3))
            S.act(rsa, psN, AF.Ln, scale=1.0 / 512, bias=EPS)
            S.act(rsa, rsa, AF.Exp, scale=-0.5)
            for h in range(4):
                S.stt(mx[:, h, :].k(h), attnT[:, h, cols], sm[:, C_GA + h:C_GA + h + 1], rsa, ALU.mult, ALU.mult)
            S.tt(sq2, hown[:, :, cols], hown[:, :, cols], ALU.mult, eng="pool")
            psN2 = S.psum(7)
            for h in range(4):
                S.mm(psN2, onesb, sq2[:, h, :], start=(h == 0), stop=(h == 3))
            S.act(rsl, psN2, AF.Ln, scale=1.0 / 512, bias=EPS)
            S.act(rsl, rsl, AF.Exp, scale=-0.5)
            for c in range(4):
                S.stt(mx[:, 4 + c, :].k(4 + c), hown[:, c, cols], sm[:, C_GL + c:C_GL + c + 1], rsl, ALU.mult, ALU.mult)

        def tv(arr, i, lo=None, hi=None):
            r = i % NR8
            v = arr[:, r, :] if lo is None else arr[:, r, lo:hi]
            return v.k(r)

        def A1(i):
            hb, s_ = i // 4, i % 4
            mx = mixedT[hb % 2]
            xb = xt[i % 3]
            S.dma("sp", xb, xo_d[i * 128:(i + 1) * 128, :])
            for n_ in range(2):
                ps = S.psum((0 if i % 2 else 2) + n_)
                for k in range(8):
                    S.mm(ps, mx[:, k, s_ * 128:(s_ + 1) * 128].k(k), woutb[:, k, n_ * 512:(n_ + 1) * 512],
                         start=(k == 0), stop=(k == 7))

        def A2(i):
            for n_ in range(2):
                ps = S.psum((0 if i % 2 else 2) + n_)
                S.tt(tmp[(i % 2) * 2 + n_], ps, g1bc[:, n_ * 512:(n_ + 1) * 512], ALU.mult)

        def A3(i):
            xb = xt[i % 3]
            x1 = x1t[i % 4]
            for n_ in range(2):
                S.tt(x1[:, n_ * 512:(n_ + 1) * 512].k(n_), tmp[(i % 2) * 2 + n_], xb[:, n_ * 512:(n_ + 1) * 512],
                     ALU.add, eng="pool")
            S.dma("sp", X1s[i * 128:(i + 1) * 128, :].k(i), x1.ks([0, 1]))

        def A4(i):
            x1f = x1t[i % 4].ks([0, 1])
            st = st2[i % 4]
            S.act(junk, x1f, AF.Square, accum_out=st[:, 0:1])
            S.act(st[:, 1:2], st[:, 0:1], AF.Ln, scale=1.0 / 1024, bias=EPS)
            S.act(st[:, 1:2], st[:, 1:2], AF.Exp, scale=-0.5)

        def A5(i):
            x1f = x1t[i % 4].ks([0, 1])
            st = st2[i % 4]
            S.stt(tmpf[i % 2], x1f, st[:, 1:2], a2bc, ALU.mult, ALU.mult)
            S.ts(diag[i % 3], identf, st[:, 1:2], None, ALU.mult)

        def A6(i):
            x1f = x1t[i % 4].ks([0, 1])
            S.tt(h2tok[i % 2], tmpf[i % 2], sh2bc, ALU.add, eng="pool")
            S.dma("sp", H2s[i * 128:(i + 1) * 128, :].k(i), h2tok[i % 2])
            for hf in range(2):
                ps = S.psum(4 + hf)
                for kk in range(4):
                    k = hf * 4 + kk
                    S.mm(ps[:, kk * 128:(kk + 1) * 128], x1f[:, k * 128:(k + 1) * 128], diag[i % 3])

        def A7(i):
            hT_ = h2Tt[i % 2]
            for hf in range(2):
                ps = S.psum(4 + hf)
                for kk in range(4):
                    k = hf * 4 + kk
                    dst = hT_[:, k, :].k(k)
                    src = ps[:, kk * 128:(kk + 1) * 128]
                    if hf == 0:
                        S.act(dst, src, AF.Identity, scale=a2[:, k:k + 1], bias=sh2[:, k:k + 1])
                    else:
                        S.ts(dst, src, a2[:, k:k + 1], sh2[:, k:k + 1], ALU.mult, ALU.add)

        def A8(i):
            hT_ = h2Tt[i % 2]
            psR = S.psum(6)
            for k in range(8):
                S.mm(psR[:, 0:36], hT_[:, k, :].k(k), wrb[:, k, :], start=(k == 0), stop=(k == 7))

        def A9(i):
            lgi = lg[i % 4]
            S.tt(lgi, S.psum(6)[:, 0:36], rows[:, R_BR:R_BR + 36], ALU.add)
            S.v("dve", "tensor_reduce", [tv(rsmA, i)], [lgi], tv(rsmA, i, 0, 1), lgi[:, 0:4], AX.X, ALU.max)
            S.ts(tv(gmaskA, i), lgi[:, 0:4], tv(rsmA, i, 0, 1), None, ALU.is_equal)
            S.ts(tv(rsmA, i, 1, 2), tv(rsmA, i, 0, 1), -1.0, None, ALU.mult)

        def A10(i):
            lgi = lg[i % 4]
            S.act(tv(gexA, i), lgi[:, 0:4], AF.Exp, bias=tv(rsmA, i, 1, 2), accum_out=tv(rsmA, i, 2, 3))

        def A11(i):
            lgi = lg[i % 4]
            r = i % NR8
            S.recip(tv(rsmA, i, 3, 4), tv(rsmA, i, 2, 3))
            S.ts(tv(penA, i), tv(gmaskA, i), 1.0, 1e30, ALU.subtract, ALU.mult)
            elm3 = V(elmA[:, r, :].ap.rearrange("p (g e) -> p g e", g=4), ("elmA", r))
            lg3 = V(lgi[:, 4:36].ap.rearrange("p (g e) -> p g e", g=4), lgi.key)
            penb = V(penA[:, r, :].ap.unsqueeze(2).to_broadcast([128, 4, 8]), ("penA", r))
            S.tt(elm3, lg3, penb, ALU.add)
            S.v("dve", "max", [tv(m8A, i)], [tv(elmA, i)], tv(m8A, i), tv(elmA, i))
            S.tt(tv(rsmA, i, 4, 5), tv(m8A, i, 0, 1), tv(m8A, i, 1, 2), ALU.subtract)

        def A12(i):
            S.act(tv(rsmA, i, 5, 6), tv(rsmA, i, 4, 5), AF.Exp, scale=-1.0)

        def A13(i):
            w1 = w12[:, i, 0:1]
            w2 = w12[:, i, 1:2]
            ee = tv(rsmA, i, 5, 6)
            gp = tv(rsmA, i, 3, 4)
            S.ts(ee, ee, 1.0, None, ALU.add)
            S.recip(ee, ee)
            S.tt(w1, ee, gp, ALU.mult)
            S.tt(w2, gp, w1, ALU.subtract)
            S.ts(ohA[:, i, :], tv(elmA, i), tv(m8A, i, 0, 1), None, ALU.is_equal)
            S.ts(ohB[:, i, :], tv(elmA, i), tv(m8A, i, 1, 2), None, ALU.is_equal)
            S.tt(tv(MiA, i), ohA[:, i, :], ohB[:, i, :], ALU.add)

        def A14(i):
            psK = S.psum(7)
            S.mm(psK[:, 0:32], ltrib, tv(MiA, i), start=True, stop=False)
            S.mm(psK[:, 0:32], onesb, Msum, start=False, stop=True)
            S.copy(rank[:, i, :], psK[:, 0:32], eng="act")
            S.tt(Msum, Msum, tv(MiA, i), ALU.add)

        stages = [A1, A2, A3, A4, A5, A6, A7, A8, A9, A10, A11, A12, A13, A14]
        SA0(0)
        for t in range(NTA + len(stages) - 1):
            for si in range(len(stages) - 1, -1, -1):
                i = t - si
                if 0 <= i < NTA:
                    if si == 0 and t % 4 == 1 and (t + 3) // 4 < NTA // 4:
                        SA0((t + 3) // 4)
                    stages[si](i)
        S.release(ma)
        S.arena_bytes += 4 * 4096 * 2
        mr = S.mark()
        cnt = S.alloc("cnt", [32], F32)
        yv = S.alloc("yv", [32], F32)
        zi = S.alloc("zi", [32], I32)
        zf = S.alloc("zf", [32], F32)
        cum = S.alloc("cum", [32], F32)
        pstart = S.alloc("pstart", [32], F32)
        ones32 = S.alloc("ones32", [32], F32)
        be = S.alloc("be", [NBLK], F32)
        chg = S.alloc("chg", [NBLK], F32)
        t1 = S.alloc("t1", [NBLK], F32)
        idxw = S.alloc("idxw", [NBLK], I32)
        dstf = S.alloc("dstf", [2, 32], F32)
        dsti = S.alloc("dsti", [2, 32], I32)
        mr2 = S.mark()
        cmp3 = S.alloc("cmp3", [NBLK, 32], F32)
        big = S.alloc("big", [32, 32], F32)
        psK = S.psum(7)
        S.mm(psK[:, 0:32], onesb, Msum)
        S.copy(cnt, psK[:, 0:32])
        S.ts(yv, cnt, 1.0 / 128, 127.0 / 128, ALU.mult, ALU.add)
        S.copy(zi, yv)
        S.copy(zf, zi)
        S.tt(yv, zf, yv, ALU.is_gt)
        S.tt(zf, zf, yv, ALU.subtract)
        S.memset(ones32, 1.0)
        S.v("dve", "tensor_tensor_scan", [cum], [ones32, zf], cum, ones32, zf, 0.0, ALU.mult, ALU.add)
        S.tt(pstart, cum, zf, ALU.subtract)
        S.ts(pstart, pstart, 128.0, None, ALU.mult)
        cumb = V(cum.ap.unsqueeze(1).to_broadcast([128, NBLK, 32]), cum.key)
        jb = V(rows[:, R_J:R_J + NBLK].ap.unsqueeze(2).to_broadcast([128, NBLK, 32]), rows.key)
        S.tt(cmp3, cumb, jb, ALU.is_le)
        S.v("dve", "tensor_reduce", [be], [cmp3], be, cmp3, AX.X, ALU.add)
        S.ts(be, be, 31.0, None, ALU.min)
        S.memset(chg, 1.0)
        S.tt(chg[:, 2:NBLK], be[:, 2:NBLK], be[:, 0:NBLK - 2], ALU.not_equal)
        S.ts(t1, be, 128.0, None, ALU.mult)
        S.ts(t1, t1, sm[:, C_PCOL:C_PCOL + 1], None, ALU.add)
        S.ts(chg, chg, -1.0e6, 1.0e6, ALU.mult, ALU.add)
        S.tt(t1, t1, chg, ALU.add)
        S.copy(idxw, t1)
        psb_ = V(pstart.ap.unsqueeze(1).to_broadcast([128, 32, 32]), pstart.key)
        S.tt(big, rank, psb_, ALU.add)
        for q_, oh in enumerate((ohA, ohB)):
            cm = V(cmp3.ap.rearrange("p a b -> p (a b)")[:, 0:1024].rearrange("p (a b) -> p a b", a=32), cmp3.key)
            S.tt(cm, big, oh, ALU.mult)
            S.v("dve", "tensor_reduce", [dstf], [cmp3], dstf[:, q_, :], cm, AX.X, ALU.add)
        S.copy(dsti, dstf)
        S.barrier()
        S.top = mr2
        if debug:
            S.dma("sp", dout("d_cnt", [128, 32]), cnt)
            S.dma("sp", dout("d_be", [128, NBLK]), be)
            S.dma("sp", dout("d_idxw", [128, NBLK], I32), idxw)
            S.dma("sp", dout("d_dst", [128, 2, 32], I32), dsti)
            S.dma("sp", dout("d_w12", [128, 32, 2]), w12)
        hb2 = [S.alloc(f"hb2_{i}", [1024], BF16) for i in range(2)]
        xg_keys = []
        for i in range(NTA):
            t_ = hb2[i % 2]
            S.dma("sp", t_, H2s[i * 128:(i + 1) * 128, :].k(i))
            for q_ in range(2):
                key = ("Xg", i, q_)
                xg_keys.append(key)

                def scat(e, src=t_.ap, ix=dsti.ap, i=i, q_=q_):
                    return e.indirect_dma_start(out=Xg_d[:, :],
                                                out_offset=bass.IndirectOffsetOnAxis(ap=ix[:, q_, i:i + 1], axis=0),
                                                in_=src, in_offset=None)
                S.dma_custom("pool", scat, reads=[t_.key, dsti.key], writes=[key])
        wgu = [S.alloc(f"wgu{i}", [8, 512], BF16) for i in range(2)]
        wdn = [S.alloc(f"wdn{i}", [2, 1024], BF16) for i in range(2)]
        Xb = [S.alloc(f"Xb{i}", [1024], BF16) for i in range(3)]
        XgT = [S.alloc(f"XgT{i}", [8, 128], BF16) for i in range(2)]
        sgl = [S.alloc(f"sgl{i}", [256], F32) for i in range(2)]
        hid = [S.alloc(f"hid{i}", [256], BF16) for i in range(2)]
        hidT = [S.alloc(f"hidT{i}", [2, 128], BF16) for i in range(2)]
        Yb = [S.alloc(f"Yb{i}", [1024], F32) for i in range(2)]
        regs = {}
        y_keys = []

        def _wgather(j, dst_v, src_d):
            def gath(e, o=dst_v.ap.rearrange("p a b -> p (a b)"), src_d=src_d, ix=idxw.ap, j=j):
                if "bc" not in regs:
                    regs["bc"] = e.alloc_register("bcreg")
                    e.reg_mov(regs["bc"], 4095)
                return e.indirect_dma_start(out=o, out_offset=None, in_=src_d[:, :],
                                            in_offset=bass.IndirectOffsetOnAxis(ap=ix[:, j:j + 1], axis=0),
                                            bounds_check=regs["bc"], oob_is_err=False)
            S.dma_custom("pool", gath, reads=[idxw.key] + wb_keys, writes=[dst_v.key])

        def B1wg(j):
            s_ = j % 2
            _wgather(j, wgu[s_][:, 0:4, :].k("lo"), wb_d[0])
            _wgather(j, wgu[s_][:, 4:8, :].k("hi"), wb_d[1])

        def B1wd(j):
            _wgather(j, wdn[j % 2], wb_d[2])

        def B1x(j):
            S.dma_custom("sp", (lambda e, o=Xb[j % 3].ap, j=j: e.dma_start(out=o, in_=Xg_d[j * 128:(j + 1) * 128, :])),
                         reads=xg_keys, writes=[Xb[j % 3].key])

        def B2(j):
            s_ = j % 2
            psX = S.psum(s_, BF16)
            for k in range(8):
                S.tr(psX[:, k * 128:(k + 1) * 128], Xb[j % 3][:, k * 128:(k + 1) * 128], identb)
            S.copy(XgT[s_], V(psX.ap.rearrange("p (k t) -> p k t", k=8), psX.key), eng=("act" if s_ == 0 else "dve"))

        def B3(j):
            s_ = j % 2
            psG = S.psum(2 + s_)
            for k in range(8):
                S.mm(psG, XgT[s_][:, k, :], wgu[s_][:, k, :].k("lo" if k < 4 else "hi"), start=(k == 0), stop=(k == 7))
            S.act(sgl[s_], psG[:, 0:256], AF.Silu)
            S.tt(hid[s_], psG[:, 256:512], sgl[s_], ALU.mult)

        def B4(j):
            s_ = j % 2
            psTt = S.psum(4, BF16)
            for c in range(2):
                S.tr(psTt[:, c * 128:(c + 1) * 128], hid[s_][:, c * 128:(c + 1) * 128], identb)
            S.copy(hidT[s_], V(psTt[:, 0:256].ap.rearrange("p (c t) -> p c t", c=2), psTt.key), eng="act")
            for n_ in range(2):
                psD = S.psum(5 + n_)
                for c in range(2):
                    S.mm(psD, hidT[s_][:, c, :], wdn[s_][:, c, n_ * 512:(n_ + 1) * 512], start=(c == 0), stop=(c == 1))
                if n_ == 0:
                    S.copy(Yb[s_][:, 0:512], psD, eng="act")
                else:
                    S.copy(Yb[s_][:, 512:1024], psD)
            key = ("Y", j)
            y_keys.append(key)
            S.dma_custom("sp", (lambda e, i_=Yb[s_].ap, j=j: e.dma_start(out=Y_d[j * 128:(j + 1) * 128, :], in_=i_)),
                         reads=[Yb[s_].key], writes=[key])

        B1wg(0)
        B1wd(0)
        B1wg(1)
        B1x(0)
        B1x(1)
        B2(0)
        for t in range(NBLK + 1):
            if t + 2 < NBLK:
                B1x(t + 2)
            if t + 1 < NBLK:
                B2(t + 1)
            if t < NBLK:
                B3(t)
                if t + 2 < NBLK:
                    B1wg(t + 2)
            if 0 <= t - 1 < NBLK:
                B4(t - 1)
            if t + 1 < NBLK:
                B1wd(t + 1)
        Y0 = [S.alloc(f"Y0_{i}", [1024], F32) for i in range(2)]
        Y1 = [S.alloc(f"Y1_{i}", [1024], F32) for i in range(2)]
        x1c = [S.alloc(f"x1c{i}", [1024], F32) for i in range(2)]
        for i in range(NTA):
            b_ = i % 2
            for q_, dstt in enumerate((Y0[b_], Y1[b_])):
                def gy(e, o=dstt.ap, ix=dsti.ap, i=i, q_=q_):
                    return e.indirect_dma_start(out=o, out_offset=None, in_=Y_d[:, :],
                                                in_offset=bass.IndirectOffsetOnAxis(ap=ix[:, q_, i:i + 1], axis=0))
                S.dma_custom("pool", gy, reads=y_keys + [dsti.key], writes=[dstt.key])
            S.dma("sp", x1c[b_], X1s[i * 128:(i + 1) * 128, :].k(i))
            S.act(Y0[b_], Y0[b_], AF.Identity, scale=w12[:, i, 0:1])
            S.stt(Y0[b_], Y1[b_], w12[:, i, 1:2], Y0[b_], ALU.mult, ALU.add)
            S.tt(Y0[b_], Y0[b_], g2bc, ALU.mult)
            S.tt(x1c[b_], x1c[b_], Y0[b_], ALU.add)
            S.dma("sp", out_d[i * 128:(i + 1) * 128, :], x1c[b_])
        S.emit()
    return nc, dbg

def prepare_inputs(inp):
    L = 0
    f32 = np.float32
    g = lambda n: np.asarray(inp[n][L], dtype=f32)
    x = np.asarray(inp["x"], dtype=f32)
    c = np.asarray(inp["c"], dtype=f32)
    pos = np.asarray(inp["positions"]).astype(np.int32)
    colT = lambda v, k: np.ascontiguousarray(v.reshape(k, 128).T)
    inv = (1.0 / (10000.0 ** (np.arange(0, 64, 2, dtype=f32) / 64.0))).astype(f32)
    shared = {
        "ident": np.eye(128, dtype=f32),
        "w_ada": g("w_ada"), "w_in": g("w_in"), "w_q_b": g("w_q_b"), "w_kv_b": g("w_kv_b"),
        "lru_wa": g("lru_wa"), "lru_wx": g("lru_wx"), "w_out": g("w_out"),
        "w_router": np.ascontiguousarray(np.concatenate([g("w_router_group"), g("w_router_expert")], axis=1)),
        "ltri": np.triu(np.ones((128, 128), f32), k=1),
    }
    wg = g("w_gate").reshape(32, 8, 128, 256)
    wu = g("w_up").reshape(32, 8, 128, 256)
    wgu = np.concatenate([wg, wu], axis=-1).transpose(0, 2, 1, 3)
    shared["wgu_lo"] = np.ascontiguousarray(wgu[:, :, 0:4, :]).reshape(4096, 2048)
    shared["wgu_hi"] = np.ascontiguousarray(wgu[:, :, 4:8, :]).reshape(4096, 2048)
    shared["wd_l"] = np.ascontiguousarray(g("w_down").reshape(32, 2, 128, 1024).transpose(0, 2, 1, 3)).reshape(4096, 2048)
    rows = np.zeros((NR,), f32)
    rows[R_GQA:R_GQA + 256] = g("q_a_norm")
    rows[R_GKVA:R_GKVA + 128] = g("kv_a_norm")
    rows[R_GQ:R_GQ + 192] = g("q_norm")
    rows[R_GKPE:R_GKPE + 64] = g("k_norm")[128:]
    rows[R_INV:R_INV + 32] = inv
    rows[R_BR:R_BR + 4] = g("b_router_group")
    rows[R_BR + 4:R_BR + 36] = g("b_router_expert")
    rows[R_J:R_J + 96] = np.arange(96, dtype=f32)
    shared["rows"] = np.ascontiguousarray(np.broadcast_to(rows[None, :], (128, NR)))
    maps = []
    for core in range(8):
        b, p = core // 2, core % 2
        sm = np.zeros((128, NS), f32)
        sm[:, C_CT:C_CT + 8] = colT(c[b], 8)
        sm[:, C_BADA:C_BADA + 48] = colT(g("b_ada"), 48)
        sm[:, C_G1:C_G1 + 8] = colT(g("norm1_g"), 8)
        sm[:, C_G2:C_G2 + 8] = colT(g("norm2_g"), 8)
        sm[:, C_KNG] = g("k_norm")[:128]
        cw = g("conv_w")
        for ch in range(4):
            for j in range(4):
                sm[:, C_CW + ch * 4 + j] = cw[j, ch * 128:(ch + 1) * 128]
        sm[:, C_CB:C_CB + 4] = colT(g("conv_b"), 4)
        sm[:, C_BA:C_BA + 4] = colT(g("lru_ba"), 4)
        sm[:, C_BX:C_BX + 4] = colT(g("lru_bx"), 4)
        sm[:, C_LAM:C_LAM + 4] = colT(g("lru_lambda"), 4)
        sm[:, C_GA:C_GA + 4] = colT(g("attn_out_norm"), 4)
        sm[:, C_GL:C_GL + 4] = colT(g("lru_out_norm"), 4)
        sm[:, C_PE] = 1.0 - p
        sm[:, C_PO] = float(p)
        sm[:, C_PCOL] = np.arange(128, dtype=f32)
        xb = x[b]
        own = xb.reshape(32, 2, 128, 1024)[:, p].reshape(4096, 1024)
        pb = pos[b]
        pown = pb.reshape(32, 2, 128)[:, p].reshape(4096)
        mask = np.zeros((128, 8, 512), f32)
        kk = np.arange(128)[:, None]
        qq = np.arange(128)[None, :]
        for r in range(8):
            for j in range(4):
                qb = 2 * j + p
                if r < qb:
                    m = np.ones((128, 128), f32)
                elif r == qb:
                    m = (kk <= qq).astype(f32)
                else:
                    m = np.zeros((128, 128), f32)
                mask[:, r, j * 128:(j + 1) * 128] = m
        d = dict(shared)
        d.update({
            "xf": np.ascontiguousarray(xb), "xo": np.ascontiguousarray(own),
            "posf_pm": np.ascontiguousarray(pb.reshape(64, 128).T),
            "poso_pm": np.ascontiguousarray(pown.reshape(32, 128).T),
            "posf_row": np.ascontiguousarray(pb.reshape(1, 8192)),
            "smalls": sm, "maskf": mask,
        })
        maps.append(d)
    return maps


def assemble(results):
    out = np.zeros((4, 8192, 1024), np.float32)
    ov = out.reshape(4, 32, 2, 128, 1024)
    for core in range(8):
        b, p = core // 2, core % 2
        ov[b, :, p] = np.asarray(results[core]["out"]).reshape(32, 128, 1024)
    return out


_NC_CACHE = {}


def kernel(**inputs):
    if "nc" not in _NC_CACHE:
        _NC_CACHE["nc"] = build_nc()[0]
    nc = _NC_CACHE["nc"]
    maps = prepare_inputs(inputs)
    res = run_bass_kernel_spmd(nc, maps, core_ids=list(range(8)))
    return assemble(res.results)
```
